# Optimizing a Trainium2 kernel written in Bass

```python
import jax, jax.numpy as jnp
from jax import lax
import numpy as np

D_MODEL = 1024
BATCH = 4
SEQ = 4096
DEPTH = 4

GRID_W = 64
CTX_LEN = 256
N_MIXERS = 3
Q_BLOCK = 128
ROPE_THETA = 10000.0
NORM_EPS = 1e-6
N_MOD = 6
MLA_HEADS = 8
MLA_Q_LORA = 512
MLA_KV_LORA = 256
MLA_NOPE = 128
MLA_ROPE = 64
MLA_V = 128
CONV_WIDTH = 3
CONV_DIM = D_MODEL
GQA_HEADS = 16
GQA_KV_HEADS = 8
GQA_HEAD_DIM = 128
MOE_GROUPS = 4
MOE_EXPERTS_PER_GROUP = 8
MOE_EXPERTS = MOE_GROUPS * MOE_EXPERTS_PER_GROUP
MOE_TOP_K = 2
MOE_FF = 512
MOE_BLOCK = 128

kernel_name = "hybrid_mla_conv_gqa_hier_moe_dit"


def _n_layers_of(kind):
    return len(range(kind, DEPTH, N_MIXERS))


def _rmsnorm(x, g):
    xf = x.astype(jnp.float32)
    y = xf * lax.rsqrt(jnp.mean(xf * xf, axis=-1, keepdims=True) + NORM_EPS)
    return (y * g.astype(jnp.float32)).astype(x.dtype)


def _modulate(x, g, shift, scale):
    return _rmsnorm(x, g) * (1 + scale) + shift


def _rope_1d(x, pos):
    half = x.shape[-1] // 2
    inv_freq = ROPE_THETA ** (-jnp.arange(half, dtype=jnp.float32) / half)
    ang = pos.astype(jnp.float32)[:, None] * inv_freq[None, :]
    cos, sin = jnp.cos(ang)[:, None, :], jnp.sin(ang)[:, None, :]
    xf = x.astype(jnp.float32)
    x1, x2 = xf[..., :half], xf[..., half:]
    return jnp.concatenate([x1 * cos - x2 * sin, x1 * sin + x2 * cos], axis=-1).astype(x.dtype)


def _axial_rope(x, rows, cols):
    half = x.shape[-1] // 2
    return jnp.concatenate([_rope_1d(x[..., :half], rows), _rope_1d(x[..., half:], cols)], axis=-1)


def _attend(q, k, v, scale):
    B, n, H, dk = q.shape
    Hk, dv = k.shape[2], v.shape[-1]
    G = H // Hk
    nb = n // Q_BLOCK
    qb = q.reshape(B, nb, Q_BLOCK, Hk, G, dk).transpose(1, 0, 2, 3, 4, 5)

    def block(qblk):
        s = jnp.einsum("bqhgd,bkhd->bhgqk", qblk, k).astype(jnp.float32) * scale
        p = jax.nn.softmax(s, axis=-1).astype(v.dtype)
        return jnp.einsum("bhgqk,bkhd->bqhgd", p, v)

    o = lax.map(block, qb)
    return o.transpose(1, 0, 2, 3, 4, 5).reshape(B, n, H * dv)


def _mla_project(a, w_dq, g_q, w_uq, w_dkv, g_kv, w_ukv, pos, want_q):
    B, n, _ = a.shape
    ckv = a @ w_dkv
    c_kv = _rmsnorm(ckv[..., :MLA_KV_LORA], g_kv)
    k_pe = ckv[..., MLA_KV_LORA:][:, :, None, :]
    kv = (c_kv @ w_ukv).reshape(B, n, MLA_HEADS, MLA_NOPE + MLA_V)
    k_nope, v = kv[..., :MLA_NOPE], kv[..., MLA_NOPE:]
    if pos is not None:
        k_pe = _axial_rope(k_pe, *pos)
    k = jnp.concatenate([k_nope, jnp.broadcast_to(k_pe, (B, n, MLA_HEADS, MLA_ROPE))], axis=-1)
    q = None
    if want_q:
        cq = _rmsnorm(a @ w_dq, g_q)
        q = (cq @ w_uq).reshape(B, n, MLA_HEADS, MLA_NOPE + MLA_ROPE)
        if pos is not None:
            q = jnp.concatenate([q[..., :MLA_NOPE], _axial_rope(q[..., MLA_NOPE:], *pos)], axis=-1)
    return q, k, v


def _mla(a_lat, a_ctx, pos, ctx_out, w_dq, g_q, w_uq, w_dkv, g_kv, w_ukv, w_o):
    scale = (MLA_NOPE + MLA_ROPE) ** -0.5
    q_l, k_l, v_l = _mla_project(a_lat, w_dq, g_q, w_uq, w_dkv, g_kv, w_ukv, pos, True)
    q_c, k_c, v_c = _mla_project(a_ctx, w_dq, g_q, w_uq, w_dkv, g_kv, w_ukv, None, ctx_out)
    k_all = jnp.concatenate([k_c, k_l], axis=1)
    v_all = jnp.concatenate([v_c, v_l], axis=1)
    y_lat = _attend(q_l, k_all, v_all, scale) @ w_o
    y_ctx = _attend(q_c, k_c, v_c, scale) @ w_o if ctx_out else None
    return y_lat, y_ctx


def _short_conv_seq(a, w_in, conv_w, w_out):
    bcu = a @ w_in
    b_gate, c_gate, u = jnp.split(bcu, 3, axis=-1)
    z = lax.conv_general_dilated(
        c_gate * u, conv_w[:, None, :], window_strides=(1,),
        padding=[((CONV_WIDTH - 1) // 2, (CONV_WIDTH - 1) // 2)],
        dimension_numbers=("NWC", "WIO", "NWC"), feature_group_count=CONV_DIM)
    return (b_gate * z) @ w_out


def _short_conv(a_lat, a_ctx, ctx_out, w_in, conv_w, w_out):
    y_lat = _short_conv_seq(a_lat, w_in, conv_w, w_out)
    y_ctx = _short_conv_seq(a_ctx, w_in, conv_w, w_out) if ctx_out else None
    return y_lat, y_ctx


def _gqa_project(a, w_qkv, q_g, k_g, pos, want_q):
    B, n, _ = a.shape
    dq = GQA_HEADS * GQA_HEAD_DIM
    dkv = GQA_KV_HEADS * GQA_HEAD_DIM
    kv = a @ w_qkv[:, dq:]
    k = _rmsnorm(kv[..., :dkv].reshape(B, n, GQA_KV_HEADS, GQA_HEAD_DIM), k_g)
    v = kv[..., dkv:].reshape(B, n, GQA_KV_HEADS, GQA_HEAD_DIM)
    if pos is not None:
        k = _axial_rope(k, *pos)
    q = None
    if want_q:
        q = _rmsnorm((a @ w_qkv[:, :dq]).reshape(B, n, GQA_HEADS, GQA_HEAD_DIM), q_g)
        if pos is not None:
            q = _axial_rope(q, *pos)
    return q, k, v


def _gqa(a_lat, a_ctx, pos, ctx_out, w_qkv, q_g, k_g, w_o):
    scale = GQA_HEAD_DIM ** -0.5
    q_l, k_l, v_l = _gqa_project(a_lat, w_qkv, q_g, k_g, pos, True)
    q_c, k_c, v_c = _gqa_project(a_ctx, w_qkv, q_g, k_g, None, ctx_out)
    k_all = jnp.concatenate([k_c, k_l], axis=1)
    v_all = jnp.concatenate([v_c, v_l], axis=1)
    y_lat = _attend(q_l, k_all, v_all, scale) @ w_o
    y_ctx = _attend(q_c, k_c, v_c, scale) @ w_o if ctx_out else None
    return y_lat, y_ctx


def _hier_moe(h, w_group, b_group, w_expert, b_expert, w_gate, w_up, w_down):
    T, D = h.shape
    hf = h.astype(jnp.float32)
    g_prob = jax.nn.softmax(hf @ w_group.astype(jnp.float32), axis=-1)
    g_sel = jnp.argmax(g_prob + b_group.astype(jnp.float32), axis=-1)
    g_w = jnp.take_along_axis(g_prob, g_sel[:, None], axis=-1)
    e_logits = (hf @ w_expert.astype(jnp.float32)).reshape(T, MOE_GROUPS, MOE_EXPERTS_PER_GROUP)
    e_prob = jax.nn.softmax(jnp.take_along_axis(e_logits, g_sel[:, None, None], axis=1)[:, 0], axis=-1)
    e_bias = b_expert.astype(jnp.float32).reshape(MOE_GROUPS, MOE_EXPERTS_PER_GROUP)[g_sel]
    _, top_idx = lax.top_k(e_prob + e_bias, MOE_TOP_K)
    top_w = jnp.take_along_axis(e_prob, top_idx, axis=-1)
    top_w = top_w / jnp.sum(top_w, axis=-1, keepdims=True) * g_w
    expert_id = g_sel[:, None] * MOE_EXPERTS_PER_GROUP + top_idx

    A = T * MOE_TOP_K
    eid = expert_id.reshape(A)
    tok = jnp.arange(A, dtype=jnp.int32) // MOE_TOP_K
    wts = top_w.reshape(A)
    order = jnp.argsort(eid)
    e_s, tok_s, w_s = eid[order], tok[order], wts[order]
    counts = jnp.bincount(eid, length=MOE_EXPERTS)
    starts = jnp.cumsum(counts) - counts
    padded = (counts + MOE_BLOCK - 1) // MOE_BLOCK * MOE_BLOCK
    pends = jnp.cumsum(padded)
    pstarts = pends - padded
    dest = pstarts[e_s] + (jnp.arange(A, dtype=jnp.int32) - starts[e_s])
    nb = -(-A // MOE_BLOCK) + MOE_EXPERTS
    xp = jnp.zeros((nb * MOE_BLOCK, D), h.dtype).at[dest].set(h[tok_s])
    blk_e = jnp.clip(jnp.searchsorted(pends, jnp.arange(nb, dtype=jnp.int32) * MOE_BLOCK, side="right"),
                     0, MOE_EXPERTS - 1)

    def expert_block(args):
        xb, e = args
        return (jax.nn.silu(xb @ w_gate[e]) * (xb @ w_up[e])) @ w_down[e]

    yp = lax.map(expert_block, (xp.reshape(nb, MOE_BLOCK, D), blk_e)).reshape(nb * MOE_BLOCK, D)
    return jax.ops.segment_sum(yp[dest] * w_s[:, None].astype(h.dtype), tok_s, num_segments=T)


def _normal(k, shape, scale):
    return jax.random.normal(k, shape, jnp.float32) * scale


def setup_inputs(seed: int = 0) -> dict:
    key = jax.random.key(seed)
    ks = iter(jax.random.split(key, 40))
    D = D_MODEL
    nA, nB, nC = _n_layers_of(0), _n_layers_of(1), _n_layers_of(2)
    dq = GQA_HEADS * GQA_HEAD_DIM
    dkv = GQA_KV_HEADS * GQA_HEAD_DIM
    return {
        "x": _normal(next(ks), (BATCH, SEQ, D), 1.0),
        "c": _normal(next(ks), (BATCH, D), 1.0),
        "ctx": _normal(next(ks), (BATCH, CTX_LEN, D), 1.0),
        "c_ctx": _normal(next(ks), (D,), 1.0),
        "w_mod": _normal(next(ks), (DEPTH, D, N_MOD * D), 0.5 * D ** -0.5),
        "b_mod": _normal(next(ks), (DEPTH, N_MOD * D), 0.02),
        "norm_mix_g": 1.0 + _normal(next(ks), (DEPTH, D), 0.05),
        "norm_ffn_g": 1.0 + _normal(next(ks), (DEPTH, D), 0.05),
        "mla_w_dq": _normal(next(ks), (nA, D, MLA_Q_LORA), D ** -0.5),
        "mla_g_q": 1.0 + _normal(next(ks), (nA, MLA_Q_LORA), 0.05),
        "mla_w_uq": _normal(next(ks), (nA, MLA_Q_LORA, MLA_HEADS * (MLA_NOPE + MLA_ROPE)), MLA_Q_LORA ** -0.5),
        "mla_w_dkv": _normal(next(ks), (nA, D, MLA_KV_LORA + MLA_ROPE), D ** -0.5),
        "mla_g_kv": 1.0 + _normal(next(ks), (nA, MLA_KV_LORA), 0.05),
        "mla_w_ukv": _normal(next(ks), (nA, MLA_KV_LORA, MLA_HEADS * (MLA_NOPE + MLA_V)), MLA_KV_LORA ** -0.5),
        "mla_w_o": _normal(next(ks), (nA, MLA_HEADS * MLA_V, D), (MLA_HEADS * MLA_V) ** -0.5),
        "conv_w_in": _normal(next(ks), (nB, D, 3 * CONV_DIM), D ** -0.5),
        "conv_w": _normal(next(ks), (nB, CONV_WIDTH, CONV_DIM), CONV_WIDTH ** -0.5),
        "conv_w_out": _normal(next(ks), (nB, CONV_DIM, D), CONV_DIM ** -0.5),
        "gqa_w_qkv": _normal(next(ks), (nC, D, dq + 2 * dkv), D ** -0.5),
        "gqa_q_norm_g": 1.0 + _normal(next(ks), (nC, GQA_HEAD_DIM), 0.05),
        "gqa_k_norm_g": 1.0 + _normal(next(ks), (nC, GQA_HEAD_DIM), 0.05),
        "gqa_w_o": _normal(next(ks), (nC, dq, D), dq ** -0.5),
        "moe_w_group": _normal(next(ks), (DEPTH, D, MOE_GROUPS), D ** -0.5),
        "moe_b_group": _normal(next(ks), (DEPTH, MOE_GROUPS), 0.01),
        "moe_w_expert": _normal(next(ks), (DEPTH, D, MOE_EXPERTS), D ** -0.5),
        "moe_b_expert": _normal(next(ks), (DEPTH, MOE_EXPERTS), 0.01),
        "moe_w_gate": _normal(next(ks), (DEPTH, MOE_EXPERTS, D, MOE_FF), D ** -0.5),
        "moe_w_up": _normal(next(ks), (DEPTH, MOE_EXPERTS, D, MOE_FF), D ** -0.5),
        "moe_w_down": _normal(next(ks), (DEPTH, MOE_EXPERTS, MOE_FF, D), MOE_FF ** -0.5),
        "final_norm_g": 1.0 + _normal(next(ks), (D,), 0.05),
    }


def reference(x, c, ctx, c_ctx, w_mod, b_mod, norm_mix_g, norm_ffn_g,
              mla_w_dq, mla_g_q, mla_w_uq, mla_w_dkv, mla_g_kv, mla_w_ukv, mla_w_o,
              conv_w_in, conv_w, conv_w_out,
              gqa_w_qkv, gqa_q_norm_g, gqa_k_norm_g, gqa_w_o,
              moe_w_group, moe_b_group, moe_w_expert, moe_b_expert, moe_w_gate, moe_w_up, moe_w_down,
              final_norm_g):
    B, S, D = x.shape
    L = ctx.shape[1]
    ROWS = S // GRID_W
    rows = jnp.repeat(jnp.arange(ROWS, dtype=jnp.int32), GRID_W)
    cols = jnp.tile(jnp.arange(GRID_W, dtype=jnp.int32), ROWS)
    pos = (rows, cols)
    cond_lat = jax.nn.silu(c)
    cond_ctx = jax.nn.silu(c_ctx)[None]
    h = ctx
    for i in range(DEPTH):
        kind, j = i % N_MIXERS, i // N_MIXERS
        ctx_out = i < DEPTH - 1
        mod_l = (cond_lat @ w_mod[i] + b_mod[i])[:, None, :]
        mod_c = (cond_ctx @ w_mod[i] + b_mod[i])[:, None, :]
        sh1, sc1, g1, sh2, sc2, g2 = jnp.split(mod_l, N_MOD, axis=-1)
        csh1, csc1, cg1, csh2, csc2, cg2 = jnp.split(mod_c, N_MOD, axis=-1)
        a_lat = _modulate(x, norm_mix_g[i], sh1, sc1)
        a_ctx = _modulate(h, norm_mix_g[i], csh1, csc1)
        if kind == 0:
            y_lat, y_ctx = _mla(a_lat, a_ctx, pos, ctx_out, mla_w_dq[j], mla_g_q[j], mla_w_uq[j],
                                mla_w_dkv[j], mla_g_kv[j], mla_w_ukv[j], mla_w_o[j])
        elif kind == 1:
            y_lat, y_ctx = _short_conv(a_lat, a_ctx, ctx_out, conv_w_in[j], conv_w[j], conv_w_out[j])
        else:
            y_lat, y_ctx = _gqa(a_lat, a_ctx, pos, ctx_out, gqa_w_qkv[j], gqa_q_norm_g[j],
                                gqa_k_norm_g[j], gqa_w_o[j])
        x = x + g1 * y_lat
        f_lat = _modulate(x, norm_ffn_g[i], sh2, sc2)
        moe_args = (moe_w_group[i], moe_b_group[i], moe_w_expert[i], moe_b_expert[i],
                    moe_w_gate[i], moe_w_up[i], moe_w_down[i])
        if ctx_out:
            h = h + cg1 * y_ctx
            f_ctx = _modulate(h, norm_ffn_g[i], csh2, csc2)
            tokens = jnp.concatenate([f_lat.reshape(B * S, D), f_ctx.reshape(B * L, D)], axis=0)
            y = _hier_moe(tokens, *moe_args)
            x = x + g2 * y[:B * S].reshape(B, S, D)
            h = h + cg2 * y[B * S:].reshape(B, L, D)
        else:
            y = _hier_moe(f_lat.reshape(B * S, D), *moe_args)
            x = x + g2 * y.reshape(B, S, D)
    return _rmsnorm(x, final_norm_g)
```

```python
import numpy as np
from contextlib import ExitStack
import concourse.bass as bass
import concourse.mybir as mybir
from concourse.bass_utils import run_bass_kernel_spmd

F32 = mybir.dt.float32
BF16 = mybir.dt.bfloat16
AF = mybir.ActivationFunctionType
ALU = mybir.AluOpType
AX = mybir.AxisListType

D = 1024
NT = 17
NTOK = NT * 128
CHUNKS = [(0, 512), (512, 512), (1024, 512), (1536, 512), (2048, 128)]
EPS = 1e-6
NKEY = 2 * NTOK
NKT = NKEY // 128


def _dsz(dt):
    if dt == F32:
        return 4
    if dt == BF16:
        return 2
    s = str(dt)
    if "32" in s:
        return 4
    if "16" in s:
        return 2
    if "64" in s:
        return 8
    return 1


def _box(ap):
    t = ap.tensor
    a = ap.ap
    dsz = _dsz(ap.dtype)
    off = int(ap.offset)
    tname = type(t).__name__
    if tname.startswith("DRam"):
        lo = off
        hi = off + sum((c - 1) * s for s, c in a) + 1
        return (t.name, 0, 1, lo * dsz, hi * dsz)
    pstep, pcnt = a[0]
    if pstep == 0:
        p0, f0 = 0, off
    else:
        p0 = off // pstep
        f0 = off - p0 * pstep
    hi = f0 + sum((c - 1) * s for s, c in a[1:]) + 1
    if tname.startswith("PSum"):
        b0 = (f0 * dsz) // 2048 * 2048
        b1 = -(-(hi * dsz) // 2048) * 2048
        return (t.name, 0, 128, b0, b1)
    return (t.name, p0, p0 + pcnt, f0 * dsz, hi * dsz)


class Prog:
    CE = ("pe", "act", "dve", "pool")
    ALLQ = ("pe", "act", "dve", "pool", "sp")

    def __init__(self, nc, stack, n_dma_sems=24):
        self.nc = nc
        self.ops = {e: [] for e in self.ALLQ}
        self.esem = {e: stack.enter_context(nc.semaphore("es_" + e)) for e in self.CE}
        self.ecnt = {e: 0 for e in self.CE}
        self.known = {e: {} for e in self.ALLQ}
        self.hist = {}
        self.dsem = {}
        self.dpos = {}
        for q in ("sp", "pool", "act", "cc"):
            n = n_dma_sems if q in ("sp", "pool") else 4
            self.dsem[q] = [[stack.enter_context(nc.semaphore("ds_%s%d" % (q, i))), 0] for i in range(n)]
            self.dpos[q] = 0
        self.nops = 0

    def _need(self, eng, tok, waits):
        key, sem, val, peng = tok
        if peng == eng and eng == "pe":
            return
        if self.known[eng].get(key, 0) >= val:
            return
        if waits.get(key, (None, 0))[1] < val:
            waits[key] = (sem, val)

    def _deps(self, eng, reads, writes):
        waits = {}
        rb = [_box(a) for a in reads]
        wb = [_box(a) for a in writes]
        for b in rb:
            for r in self.hist.get(b[0], ()):
                if r[4] == "w" and r[0] < b[2] and b[1] < r[1] and r[2] < b[4] and b[3] < r[3]:
                    self._need(eng, r[5], waits)
        for b in wb:
            for r in self.hist.get(b[0], ()):
                if r[0] < b[2] and b[1] < r[1] and r[2] < b[4] and b[3] < r[3]:
                    self._need(eng, r[5], waits)
        return waits, rb, wb

    def _record(self, eng, tok, rb, wb):
        for b in wb:
            h = self.hist.setdefault(b[0], [])
            h[:] = [r for r in h if not (b[1] <= r[0] and r[1] <= b[2] and b[3] <= r[2] and r[3] <= b[4])]
            h.append([b[1], b[2], b[3], b[4], "w", tok, eng])
        for b in rb:
            h = self.hist.setdefault(b[0], [])
            if tok[3] == eng:
                h[:] = [r for r in h if not (r[4] == "r" and r[5][3] == eng
                                             and b[1] <= r[0] and r[1] <= b[2] and b[3] <= r[2] and r[3] <= b[4])]
            h.append([b[1], b[2], b[3], b[4], "r", tok, eng])

    def op(self, eng, fn, reads=(), writes=()):
        waits, rb, wb = self._deps(eng, reads, writes)
        self.ecnt[eng] += 1
        tok = ("e_" + eng, self.esem[eng], self.ecnt[eng], eng)
        wl = list(waits.items())
        for key, (sem, val) in wl:
            self.known[eng][key] = val
        self.ops[eng].append(([w for _, w in wl], fn, self.esem[eng], 1))
        self._record(eng, tok, rb, wb)
        self.nops += 1
        return tok

    def dma(self, q, out, in_, **kw):
        waits, rb, wb = self._deps(q, [in_], [out])
        idx = self.dpos[q] % len(self.dsem[q])
        slot = self.dsem[q][idx]
        self.dpos[q] += 1
        key = "d_%s%d" % (q, idx)
        sem = slot[0]
        if slot[1] > 0:
            self._need(q, (key, sem, slot[1], "dma"), waits)
        slot[1] += 16
        tok = (key, sem, slot[1], "dma")
        wl = list(waits.items())
        for k, (s, v) in wl:
            self.known[q][k] = v

        def fn(e, out=out, in_=in_, kw=kw):
            return e.dma_start(out=out, in_=in_, **kw)

        self.ops[q].append(([w for _, w in wl], fn, sem, 16))
        self._record(q, tok, rb, wb)
        self.nops += 1
        return tok

    def custom_dma(self, q, fn, reads, writes, inc=16, pool=None):
        pool = pool or q
        waits, rb, wb = self._deps(q, reads, writes)
        idx = self.dpos[pool] % len(self.dsem[pool])
        slot = self.dsem[pool][idx]
        self.dpos[pool] += 1
        key = "d_%s%d" % (pool, idx)
        sem = slot[0]
        if slot[1] > 0:
            self._need(q, (key, sem, slot[1], "dma"), waits)
        slot[1] += inc
        tok = (key, sem, slot[1], "dma")
        wl = list(waits.items())
        for k, (s_, v) in wl:
            self.known[q][k] = v
        self.ops[q].append(([w for _, w in wl], fn, sem, inc))
        self._record(q, tok, rb, wb)
        self.nops += 1
        return tok

    def wait_tok(self, eng, tok):
        key, sem, val, peng = tok
        if self.known[eng].get(key, 0) >= val:
            return
        self.known[eng][key] = val
        self.ops[eng].append(([(sem, val)], None, None, 0))

    def barrier(self):
        toks = [("e_" + e, self.esem[e], self.ecnt[e], e) for e in self.CE if self.ecnt[e] > 0]
        for q in self.dsem:
            for i, slot in enumerate(self.dsem[q]):
                if slot[1] > 0:
                    toks.append(("d_%s%d" % (q, i), slot[0], slot[1], "dma"))
        for q in self.ALLQ:
            for tk in toks:
                if tk[3] == q:
                    continue
                self.wait_tok(q, tk)
        self.hist = {}

    def emit(self):
        nc = self.nc
        engmap = {"pe": "tensor", "act": "scalar", "dve": "vector", "pool": "gpsimd", "sp": "sync"}
        with nc.Block() as block:
            for q in self.ALLQ:
                lst = self.ops[q]
                if not lst:
                    continue

                def body(e, lst=lst):
                    for waits, fn, sem, inc in lst:
                        for s, v in waits:
                            e.wait_ge(s, v)
                        if fn is not None:
                            fn(e).then_inc(sem, inc)

                getattr(block, engmap[q])(body)


class Env:
    def __init__(self, nc, st):
        self.nc = nc
        self.st = st
        self.P = Prog(nc, st)
        self._n = 0
        self.cur = st
        self.dram = {}

    def sb(self, name, shape, dt):
        return self.cur.enter_context(self.nc.sbuf_tensor("sb%d_%s" % (self._n, name), list(shape), dt))[:]

    def phase(self):
        env = self

        class _Ph:
            def __enter__(self_):
                env._n += 1
                self_.prev = env.cur
                self_.stk = ExitStack()
                self_.stk.__enter__()
                env.cur = self_.stk
                return env

            def __exit__(self_, *a):
                env.P.barrier()
                env.cur = self_.prev
                self_.stk.__exit__(None, None, None)
                return False

        return _Ph()

    def ps(self, name, shape, dt=F32):
        return self.st.enter_context(self.nc.psum_tensor(name, list(shape), dt))[:]

    def din(self, name, shape, dt=F32):
        if name not in self.dram:
            self.dram[name] = self.nc.dram_tensor(name, list(shape), dt, kind="ExternalInput").ap()
        return self.dram[name]

    def dout(self, name, shape, dt=F32):
        if name not in self.dram:
            self.dram[name] = self.nc.dram_tensor(name, list(shape), dt, kind="ExternalOutput").ap()
        return self.dram[name]

    def dscr(self, name, shape, dt=F32):
        if name not in self.dram:
            self.dram[name] = self.nc.dram_tensor(name, list(shape), dt, kind="Internal").ap()
        return self.dram[name]


def aview(t, boff, shape, dt):
    n = int(np.prod(shape)) * _dsz(dt)
    assert boff % 4 == 0
    ap = t[:, boff // 2:(boff + n) // 2]
    if dt != BF16:
        ap = ap.bitcast(dt)
    if len(shape) == 2:
        ap = ap.rearrange("p (a b) -> p a b", a=shape[0])
    elif len(shape) == 3:
        ap = ap.rearrange("p (a b c) -> p a b c", a=shape[0], b=shape[1])
    return ap


PAIRS = [[0, 1], [2, 3], [4, 5], [6, 7]]


def ALLGATHER(P, out, in_):
    P.custom_dma("pool", lambda e: e.collective_compute("AllGather", ALU.bypass, replica_groups=PAIRS, ins=[in_], outs=[out]),
                 reads=[in_], writes=[out], inc=1, pool="cc")


def bc(ap, shape):
    return ap.broadcast_to(list(shape))


def TT(P, eng, out, in0, in1, op):
    P.op(eng, lambda e: e.tensor_tensor(out=out, in0=in0, in1=in1, op=op), reads=[in0, in1], writes=[out])


def TS(P, eng, out, in0, s1, op0, s2=None, op1=None):
    rd = [in0] + [s for s in (s1, s2) if not isinstance(s, (int, float, type(None)))]
    if op1 is None:
        P.op(eng, lambda e: e.tensor_scalar(out=out, in0=in0, scalar1=s1, scalar2=None, op0=op0), reads=rd, writes=[out])
    else:
        P.op(eng, lambda e: e.tensor_scalar(out=out, in0=in0, scalar1=s1, scalar2=s2, op0=op0, op1=op1), reads=rd, writes=[out])


def STT(P, eng, out, in0, scalar, in1, op0, op1):
    rd = [in0, in1] + ([] if isinstance(scalar, (int, float)) else [scalar])
    P.op(eng, lambda e: e.scalar_tensor_tensor(out=out, in0=in0, scalar=scalar, in1=in1, op0=op0, op1=op1), reads=rd, writes=[out])


def RED(P, eng, out, in_, op):
    P.op(eng, lambda e: e.tensor_reduce(out=out, in_=in_, axis=AX.X, op=op), reads=[in_], writes=[out])


def ACT(P, out, in_, func, bias=None, scale=None, accum_out=None):
    rd = [in_]
    kw = {}
    if bias is not None:
        kw["bias"] = bias
        if not isinstance(bias, (int, float)):
            rd.append(bias)
    if scale is not None:
        kw["scale"] = scale
        if not isinstance(scale, (int, float)):
            rd.append(scale)
    wr = [out]
    if accum_out is not None:
        kw["accum_out"] = accum_out
        wr.append(accum_out)
    P.op("act", lambda e: e.activation(out=out, in_=in_, func=func, **kw), reads=rd, writes=wr)


def COPY(P, eng, out, in_):
    if eng == "act":
        P.op("act", lambda e: e.copy(out=out, in_=in_), reads=[in_], writes=[out])
    else:
        P.op(eng, lambda e: e.tensor_copy(out=out, in_=in_), reads=[in_], writes=[out])


def MM(P, out, lhsT, rhs, start, stop):
    P.op("pe", lambda e: e.matmul(out, lhsT=lhsT, rhs=rhs, start=start, stop=stop), reads=[lhsT, rhs], writes=[out])


def TR(P, out, in_, ident):
    P.op("pe", lambda e: e.transpose(out=out, in_=in_, identity=ident), reads=[in_, ident], writes=[out])


def MEMSET(P, eng, ap, val):
    P.op(eng, lambda e: e.memset(ap, val), writes=[ap])


def setup_common(E):
    P = E.P
    E.X = E.sb("X", [128, NT, D], F32)
    E.ident = E.sb("ident", [128, 128], F32)
    E.ones_f = E.sb("ones_f", [128, 128], F32)
    E.cond = E.sb("cond", [128, 8, 2], F32)
    E.scond = E.sb("scond", [128, 8, 2], F32)
    E.pa = [E.ps("pa%d" % i, [128, 512]) for i in range(2)]
    E.pb = [E.ps("pb%d" % i, [128, 512]) for i in range(2)]
    E.py = [E.ps("py%d" % i, [128, 1024]) for i in range(2)]
    E.d_ident = E.din("ident_in", [128, 128])
    E.d_cond = E.din("cond_in", [128, 8, 2])
    P.dma("sp", E.ident, E.d_ident)
    P.dma("sp", E.cond, E.d_cond)
    MEMSET(P, "dve", E.ones_f, 1.0)
    ACT(P, E.scond, E.cond, AF.Silu)


def load_x(E, d_x):
    for t in range(NT):
        E.P.dma("sp", E.X[:, t, :], d_x[t * 128:(t + 1) * 128, :])


def store_x(E, d_y):
    toks = []
    for t in range(NT):
        toks.append(E.P.dma("sp", d_y[t * 128:(t + 1) * 128, :], E.X[:, t, :]))
    for tk in toks:
        E.P.wait_tok("sp", tk)


def emit_mod(E, d_wmod, bmodT, jc0, njc, wm, modT):
    P = E.P
    wsrc = d_wmod.rearrange("(c p) j -> p c j", p=128)
    ps = E.pa[0]
    for b in range(njc // 4):
        P.dma("sp", wm, wsrc[:, :, (jc0 + 4 * b) * 128:(jc0 + 4 * b + 4) * 128])
        for jj in range(4):
            j = 4 * b + jj
            for c in range(8):
                MM(P, ps[:, 2 * j:2 * j + 2], wm[:, c, jj * 128:(jj + 1) * 128], E.scond[:, c, :], c == 0, c == 7)
    for j in range(njc):
        TS(P, "dve", modT[:, j, :], ps[:, 2 * j:2 * j + 2], bmodT[:, jc0 + j:jc0 + j + 1], ALU.add)


def emit_rowbc(E, vec, row, diag):
    P = E.P
    ps = E.py[0]
    for c in range(8):
        dg = diag[:, c % 2, :]
        TS(P, "dve", dg, E.ident, vec[:, c:c + 1], ALU.mult)
        MM(P, ps[:, c * 128:(c + 1) * 128], E.ones_f, dg, True, True)
    COPY(P, "act", row, ps[:])


def std_tiles(E, LG=None):
    tl = []
    for t in range(NT):
        n = 0 if t < 16 else 1
        tl.append(dict(src=E.X[:, t, :], dst=E.AT[:, :, t * 128:(t + 1) * 128], groups=[(0, 128, n)],
                       lg=None if LG is None else LG[:, t, :]))
    return tl


def emit_norm(E, tiles, gs, sh, scr, Wr=None):
    P = E.P
    ntl = len(tiles)
    junk = scr(0, [1024], BF16)
    ss = scr(2048, [ntl], F32)
    ms = scr(2048 + 128, [ntl], F32)
    sq = scr(2048 + 256, [ntl], F32)
    rstd = scr(2048 + 384, [ntl], F32)
    xn = [scr(4096 + i * 4096, [1024], F32) for i in range(2)]
    ft = [scr(12288 + i * 4096, [8, 128], F32) for i in range(2)]
    for t, tl in enumerate(tiles):
        ACT(P, junk, tl["src"], AF.Square, accum_out=ss[:, t:t + 1])
    TS(P, "dve", ms, ss, 1.0 / D, ALU.mult, EPS, ALU.add)
    ACT(P, sq, ms, AF.Sqrt)
    P.op("dve", lambda e: e.reciprocal(out=rstd, in_=sq), reads=[sq], writes=[rstd])
    def st1(t, tl):
        x_n = xn[t % 2]
        TS(P, "dve", x_n, tl["src"], rstd[:, t:t + 1], ALU.mult)
        ps = E.py[t % 2]
        for c in range(8):
            TR(P, ps[:, c * 128:(c + 1) * 128], x_n[:, c * 128:(c + 1) * 128], E.ident)

    def st2(t, tl):
        f_t = ft[t % 2]
        ps = E.py[t % 2]
        ncol = 0
        for (c0, c1, n) in tl["groups"]:
            ncol = max(ncol, c1)
            for c in range(8):
                ACT(P, f_t[:, c, c0:c1], ps[:, c * 128 + c0:c * 128 + c1], AF.Identity, bias=sh[:, c, n:n + 1], scale=gs[:, c, n:n + 1])
        COPY(P, "pool", tl["dst"], f_t[:, :, 0:ncol])
        if tl.get("lg") is not None:
            pl = E.pa[t % 2]
            for c in range(8):
                MM(P, pl[:, 0:36], f_t[:, c, :], Wr[:, c, :], c == 0, c == 7)
            COPY(P, "dve", tl["lg"], pl[:, 0:36])

    for t, tl in enumerate(tiles):
        st1(t, tl)
        if t > 0:
            st2(t - 1, tiles[t - 1])
    st2(len(tiles) - 1, tiles[-1])


def emit_routing(E, LG, bg, be, C, scr):
    P = E.P
    T = NT
    o = [0]

    def al(shape):
        n = int(np.prod(shape)) * 4
        a = scr(o[0], shape, F32)
        o[0] += (n + 31) // 32 * 32
        return a

    gl = LG[:, :, 0:4]
    el4 = LG[:, :, 4:36].rearrange("p t (g e) -> p t g e", g=4)
    v1 = al([T]); v2 = al([T]); v3 = al([T])
    g4a = al([T, 4]); g4b = al([T, 4]); goh = al([T, 4])
    p48 = al([T, 4, 8])
    e8a = al([T, 8]); e8b = al([T, 8]); ep = al([T, 8]); oh1 = al([T, 8]); oh2 = al([T, 8])
    gw = al([T])
    assert o[0] <= 20480, o[0]

    def b3(v, k):
        return bc(v.unsqueeze(2), [128, T, k])

    RED(P, "dve", v1, gl, ALU.max)
    TT(P, "dve", g4a, gl, b3(v1, 4), ALU.subtract)
    ACT(P, g4a, g4a, AF.Exp)
    RED(P, "dve", v2, g4a, ALU.add)
    P.op("dve", lambda e: e.reciprocal(out=v3, in_=v2), reads=[v2], writes=[v3])
    TT(P, "dve", g4a, g4a, b3(v3, 4), ALU.mult)
    TT(P, "dve", g4b, g4a, bc(bg.unsqueeze(1), [128, T, 4]), ALU.add)
    RED(P, "dve", v1, g4b, ALU.max)
    TT(P, "dve", goh, g4b, b3(v1, 4), ALU.is_equal)
    TT(P, "dve", g4b, goh, g4a, ALU.mult)
    RED(P, "dve", gw, g4b, ALU.add)
    TT(P, "dve", p48, el4, bc(goh.unsqueeze(3), [128, T, 4, 8]), ALU.mult)
    RED(P, "dve", e8a, p48.rearrange("p t g e -> p t e g"), ALU.add)
    RED(P, "dve", v1, e8a, ALU.max)
    TT(P, "dve", e8a, e8a, b3(v1, 8), ALU.subtract)
    ACT(P, e8a, e8a, AF.Exp)
    RED(P, "dve", v2, e8a, ALU.add)
    P.op("dve", lambda e: e.reciprocal(out=v3, in_=v2), reads=[v2], writes=[v3])
    TT(P, "dve", ep, e8a, b3(v3, 8), ALU.mult)
    TT(P, "dve", p48, bc(goh.unsqueeze(3), [128, T, 4, 8]), bc(be.rearrange("p (g e) -> p g e", g=4).unsqueeze(1), [128, T, 4, 8]), ALU.mult)
    RED(P, "dve", e8b, p48.rearrange("p t g e -> p t e g"), ALU.add)
    TT(P, "dve", e8b, e8b, ep, ALU.add)
    RED(P, "dve", v1, e8b, ALU.max)
    TT(P, "dve", oh1, e8b, b3(v1, 8), ALU.is_equal)
    STT(P, "dve", e8b, oh1, -1e30, e8b, ALU.mult, ALU.add)
    RED(P, "dve", v1, e8b, ALU.max)
    TT(P, "dve", oh2, e8b, b3(v1, 8), ALU.is_equal)
    TT(P, "dve", oh1, oh1, oh2, ALU.add)
    TT(P, "dve", e8a, oh1, ep, ALU.mult)
    RED(P, "dve", v2, e8a, ALU.add)
    P.op("dve", lambda e: e.reciprocal(out=v3, in_=v2), reads=[v2], writes=[v3])
    TT(P, "dve", v3, v3, gw, ALU.mult)
    TT(P, "dve", e8a, e8a, b3(v3, 8), ALU.mult)
    C4 = C.rearrange("p t (g e) -> p t g e", g=4)
    TT(P, "dve", C4, bc(goh.unsqueeze(3), [128, T, 4, 8]), bc(e8a.unsqueeze(2), [128, T, 4, 8]), ALU.mult)


def emit_experts(E, d_wg, d_wu, d_wd, WR, C, g2row, actT, sbuf_s, tmpb, nexp=32, chunks=None, first=None):
    P = E.P
    NS = WR.shape[1]
    CH = CHUNKS if chunks is None else chunks

    def wslot(m):
        return WR[:, m % NS, :]

    def load(e):
        g = wslot(3 * e).rearrange("p (c f) -> p c f", c=8)
        u = wslot(3 * e + 1).rearrange("p (c f) -> p c f", c=8)
        d = wslot(3 * e + 2).rearrange("p (c f) -> p c f", c=4)
        P.dma("pool", g, d_wg[e].rearrange("(c p) f -> p c f", p=128))
        P.dma("pool", u, d_wu[e].rearrange("(c p) f -> p c f", p=128))
        P.dma("pool", d, d_wd[e].rearrange("(c p) f -> p c f", p=128))
        return g, u, d

    pend = []
    cnt = [0]

    def gu(e, ci, g, u):
        t0, n = CH[ci]
        k = cnt[0] % 2
        a_t = actT[k]
        for fc in range(4):
            pg = E.pa[fc % 2]
            pu = E.pb[fc % 2]
            for c in range(8):
                MM(P, pg[:, 0:n], g[:, c, fc * 128:(fc + 1) * 128], E.AT[:, c, t0:t0 + n], c == 0, c == 7)
            for c in range(8):
                MM(P, pu[:, 0:n], u[:, c, fc * 128:(fc + 1) * 128], E.AT[:, c, t0:t0 + n], c == 0, c == 7)
            s = sbuf_s[fc % 2]
            ACT(P, s[:, 0:n], pg[:, 0:n], AF.Silu)
            TT(P, "dve", a_t[:, fc, 0:n], s[:, 0:n], pu[:, 0:n], ALU.mult)
        cnt[0] += 1
        return a_t

    ycnt = [0]

    def down(e, ci, d, a_t):
        t0, n = CH[ci]
        for tt in range(n // 128):
            tile = t0 // 128 + tt
            nn = 0 if tile < 16 else 1
            k = ycnt[0] % 2
            ycnt[0] += 1
            py = E.py[k]
            for half in range(2):
                for fc in range(4):
                    MM(P, py[:, half * 512:(half + 1) * 512], a_t[:, fc, tt * 128:(tt + 1) * 128], d[:, fc, half * 512:(half + 1) * 512], fc == 0, fc == 3)
            tm = tmpb[k]
            STT(P, "dve", tm, py[:], C[:, tile, e:e + 1], g2row[:, nn, :], ALU.mult, ALU.mult)
            TT(P, "dve", E.X[:, tile, :], E.X[:, tile, :], tm, ALU.add)

    if first == "prefetch":
        return load(0)
    nxt = load(0) if first is None else first
    for e in range(nexp):
        g, u, d = nxt
        for ci in range(len(CH)):
            a_t = gu(e, ci, g, u)
            if pend:
                pend.pop(0)()
            pend.append(lambda e=e, ci=ci, d=d, a_t=a_t: down(e, ci, d, a_t))
            if ci == 0 and e + 1 < nexp:
                nxt = load(e + 1)
    while pend:
        pend.pop(0)()


def phase_moe(E, d, nexp=32, chunks=None):
    P = E.P
    E.AT = E.sb("AT", [128, 8, NTOK], BF16)
    bmodT = E.sb("bmodT", [128, 48], F32)
    gffn = E.sb("gffn", [128, 8], F32)
    Wr = E.sb("Wr", [128, 8, 36], F32)
    bg = E.sb("bg", [128, 4], F32)
    be = E.sb("be", [128, 32], F32)
    for t, s in ((bmodT, d["bmodT"]), (gffn, d["gffn"]), (Wr, d["wr"]), (bg, d["bg"]), (be, d["be"])):
        P.dma("sp", t[:], s)
    WR = E.sb("WR", [128, 6, 4096], BF16)
    SCR = E.sb("SCR", [128, 10240], BF16)
    modT = E.sb("modT", [128, 24, 2], F32)
    gs2 = E.sb("gs2", [128, 8, 2], F32)
    LG = E.sb("LG", [128, NT, 36], F32)
    C = E.sb("C", [128, NT, 32], F32)
    g2row = E.sb("g2row", [128, 2, 1024], F32)
    diag = E.sb("diag", [128, 2, 128], F32)
    actT = [E.sb("actT%d" % i, [128, 4, 512], BF16) for i in range(2)]
    sbuf_s = [E.sb("ssilu%d" % i, [128, 512], BF16) for i in range(2)]
    tmpb = [E.sb("tmpb%d" % i, [128, 1024], F32) for i in range(2)]

    def scr(boff, shape, dt):
        return aview(SCR, boff, shape, dt)

    wm = aview(SCR, 0, [8, 512], F32)
    first = emit_experts(E, d["w_gate"], d["w_up"], d["w_down"], WR, C[:], g2row, actT, sbuf_s, tmpb, first="prefetch")
    emit_mod(E, d["w_mod"], bmodT, 24, 24, wm, modT)
    TS(P, "dve", gs2[:], modT[:, 8:16, :], 1.0, ALU.add)
    TT(P, "dve", gs2[:], gs2[:], bc(gffn[:].unsqueeze(2), [128, 8, 2]), ALU.mult)
    for n in range(2):
        emit_rowbc(E, modT[:, 16:24, n], g2row[:, n, :], diag)
    if "dbg" in d:
        P.dma("sp", d["dbg"][:, 0:48], modT.rearrange("p a b -> p (a b)"))
        P.dma("sp", d["dbg"][:, 48:64], gs2.rearrange("p a b -> p (a b)"))
        P.dma("sp", d["dbg"][:, 64:2112], g2row.rearrange("p a b -> p (a b)"))
    emit_norm(E, std_tiles(E, LG), gs2, modT[:, 0:8, :], scr, Wr=Wr)
    if "dbg" in d:
        P.dma("sp", d["dbg"][:, 2112:2112 + NT * 36], LG.rearrange("p a b -> p (a b)"))
    emit_routing(E, LG[:], bg[:], be[:], C[:], scr)
    if "dbg" in d:
        P.dma("sp", d["dbg"][:, 2724:2724 + NT * 32], C.rearrange("p a b -> p (a b)"))
    emit_experts(E, d["w_gate"], d["w_up"], d["w_down"], WR, C[:], g2row, actT, sbuf_s, tmpb, nexp=nexp, chunks=chunks, first=first)


def mix_prologue(E, d, scr_t, want_g1row=True):
    P = E.P
    bmodT = E.sb("bmodT", [128, 48], F32)
    gmix = E.sb("gmix", [128, 8], F32)
    P.dma("sp", bmodT, d["bmodT"])
    P.dma("sp", gmix, d["gmix"])
    modT = E.sb("modT1", [128, 24, 2], F32)
    gs1 = E.sb("gs1", [128, 8, 2], F32)
    wm = aview(scr_t, 0, [8, 512], F32)
    emit_mod(E, d["w_mod"], bmodT, 0, 24, wm, modT)
    TS(P, "dve", gs1, modT[:, 8:16, :], 1.0, ALU.add)
    TT(P, "dve", gs1, gs1, bc(gmix.unsqueeze(2), [128, 8, 2]), ALU.mult)
    g1row = None
    if want_g1row:
        g1row = E.sb("g1row", [128, 2, 1024], F32)
        diag = E.sb("diag1", [128, 2, 128], F32)
        for n in range(2):
            emit_rowbc(E, modT[:, 16:24, n], g1row[:, n, :], diag)
    return modT, gs1, g1row


def emit_resid(E, tile, py, grow, tmp):
    P = E.P
    nn = 0 if tile < 16 else 1
    TT(P, "dve", tmp, py, grow[:, nn, :], ALU.mult)
    TT(P, "dve", E.X[:, tile, :], E.X[:, tile, :], tmp, ALU.add)


def phase_conv(E, d):
    P = E.P
    E.AT = E.sb("AT", [128, 8, NTOK], BF16)
    SCR = E.sb("SCRc", [128, 10240], BF16)

    def scr(boff, shape, dt):
        return aview(SCR, boff, shape, dt)

    cw = E.sb("cw", [128, 8, 3], F32)
    hmask = E.sb("hmask", [128, 4], F32)
    XH = E.sb("XH", [128, 1024], F32)
    ATh = E.sb("ATh", [128, 8, 4], BF16)
    P.dma("sp", cw, d["cw"])
    P.dma("sp", hmask, d["hmask"])
    MEMSET(P, "dve", XH, 0.0)
    for i, src in enumerate(d["x_halo_rows"]):
        P.dma("sp", XH[i:i + 1, :], src)
    modT, gs1, g1row = mix_prologue(E, d, SCR)
    tiles = std_tiles(E) + [dict(src=XH, dst=ATh, groups=[(0, 2, 0), (2, 4, 1)])]
    emit_norm(E, tiles, gs1, modT[:, 0:8, :], scr)

    wout = E.sb("wout", [128, 8, 1024], BF16)
    P.dma("pool", wout, d["w_out"].rearrange("(c p) j -> p c j", p=128))
    bzT = E.sb("bzT", [128, 8, NTOK], BF16)
    win = [E.sb("win%d" % i, [128, 3, 8, 128], BF16) for i in range(2)]
    tmp = E.sb("tmpc", [128, 1024], F32)
    CU = scr(0, [2180], F32)
    bsb = scr(8736, [NTOK], BF16)
    csb = [scr(13088 + i * 2048, [512], F32) for i in range(2)]
    zc = scr(17184, [512], F32)
    chh = scr(19232, [4], F32)
    cuh = scr(19264, [4], F32)
    wsrc = d["w_in"].rearrange("(c p) j -> p c j", p=128)
    for fc in range(8):
        wb = win[fc % 2]
        for k in range(3):
            P.dma("pool", wb[:, k, :, :], wsrc[:, :, k * 1024 + fc * 128:k * 1024 + (fc + 1) * 128])
        ph = E.pa[0]
        for c in range(8):
            MM(P, ph[:, 0:4], wb[:, 1, c, :], ATh[:, c, :], c == 0, c == 7)
        for c in range(8):
            MM(P, ph[:, 4:8], wb[:, 2, c, :], ATh[:, c, :], c == 0, c == 7)
        COPY(P, "act", chh, ph[:, 0:4])
        TT(P, "dve", cuh, chh, ph[:, 4:8], ALU.mult)
        TT(P, "dve", cuh, cuh, hmask, ALU.mult)
        COPY(P, "dve", CU[:, 0:1], cuh[:, 0:1])
        COPY(P, "dve", CU[:, 2049:2051], cuh[:, 1:3])
        COPY(P, "dve", CU[:, 2179:2180], cuh[:, 3:4])
        for ci, (t0, n) in enumerate(CHUNKS):
            pb_ = E.pa[1]
            pc_ = E.pb[ci % 2]
            pu_ = E.py[ci % 2]
            for c in range(8):
                MM(P, pb_[:, 0:n], wb[:, 0, c, :], E.AT[:, c, t0:t0 + n], c == 0, c == 7)
            for c in range(8):
                MM(P, pc_[:, 0:n], wb[:, 1, c, :], E.AT[:, c, t0:t0 + n], c == 0, c == 7)
            for c in range(8):
                MM(P, pu_[:, 0:n], wb[:, 2, c, :], E.AT[:, c, t0:t0 + n], c == 0, c == 7)
            COPY(P, "act", bsb[:, t0:t0 + n], pb_[:, 0:n])
            cs = csb[ci % 2]
            COPY(P, "act", cs[:, 0:n], pc_[:, 0:n])
            o = 1 + t0 if ci < 4 else 2051
            TT(P, "dve", CU[:, o:o + n], cs[:, 0:n], pu_[:, 0:n], ALU.mult)
        for ci, (t0, n) in enumerate(CHUNKS):
            o = 1 + t0 if ci < 4 else 2051
            TS(P, "dve", zc[:, 0:n], CU[:, o:o + n], cw[:, fc, 1:2], ALU.mult)
            STT(P, "dve", zc[:, 0:n], CU[:, o - 1:o - 1 + n], cw[:, fc, 0:1], zc[:, 0:n], ALU.mult, ALU.add)
            STT(P, "dve", zc[:, 0:n], CU[:, o + 1:o + 1 + n], cw[:, fc, 2:3], zc[:, 0:n], ALU.mult, ALU.add)
            TT(P, "dve", bzT[:, fc, t0:t0 + n], bsb[:, t0:t0 + n], zc[:, 0:n], ALU.mult)
    for t in range(NT):
        py = E.py[t % 2]
        for half in range(2):
            for fc in range(8):
                MM(P, py[:, half * 512:(half + 1) * 512], bzT[:, fc, t * 128:(t + 1) * 128], wout[:, fc, half * 512:(half + 1) * 512], fc == 0, fc == 7)
        emit_resid(E, t, py, g1row, tmp)


def emit_rope(E, out, x, C2, S2, nh, half, tmp1, tmp2):
    P = E.P
    hd = 4 * half
    xv = x.rearrange("p (h a j f) -> p h a j f", h=nh, a=2, j=2)
    t2 = tmp2.rearrange("p (h a j f) -> p h a j f", h=nh, a=2, j=2)
    Cb = bc(C2.unsqueeze(1), [128, nh, hd])
    Sv = S2.rearrange("p (a j f) -> p a j f", a=2, j=2)
    TT(P, "dve", tmp1.rearrange("p (h d) -> p h d", h=nh), x.rearrange("p (h d) -> p h d", h=nh), Cb, ALU.mult)
    for j in range(2):
        TT(P, "pool", t2[:, :, :, j, :], xv[:, :, :, 1 - j, :], bc(Sv[:, :, j, :].unsqueeze(1), [128, nh, 2, half]), ALU.mult)
    TT(P, "dve", out, tmp1, tmp2, ALU.add)


def emit_headnorm(E, qn, ps, nh, hd, grow, scr4):
    P = E.P
    sq, ss, rs = scr4
    ACT(P, qn, ps, AF.Copy)
    ACT(P, sq, ps, AF.Square)
    RED(P, "dve", ss, sq.rearrange("p (h d) -> p h d", h=nh), ALU.add)
    TS(P, "dve", ss, ss, 1.0 / hd, ALU.mult, EPS, ALU.add)
    ACT(P, rs, ss, AF.Sqrt)
    P.op("dve", lambda e: e.reciprocal(out=ss, in_=rs), reads=[rs], writes=[ss])
    q3 = qn.rearrange("p (h d) -> p h d", h=nh)
    TT(P, "dve", q3, q3, bc(ss.unsqueeze(2), [128, nh, hd]), ALU.mult)
    TT(P, "dve", q3, q3, bc(grow.unsqueeze(1), [128, nh, hd]), ALU.mult)


def phase_gqa_proj(E, d):
    P = E.P
    E.AT = E.sb("AT", [128, 8, NTOK], BF16)
    SCR = E.sb("SCRa", [128, 10240], BF16)

    def scr(boff, shape, dt):
        return aview(SCR, boff, shape, dt)

    modT, gs1, _ = mix_prologue(E, d, SCR, want_g1row=False)
    emit_norm(E, std_tiles(E), gs1, modT[:, 0:8, :], scr)
    identb = E.sb("identb", [128, 128], BF16)
    COPY(P, "dve", identb, E.ident)
    gq = E.sb("gq", [128, 128], F32)
    gk = E.sb("gk", [128, 128], F32)
    P.dma("sp", gq, d["gq"])
    P.dma("sp", gk, d["gk"])
    wb = [E.sb("wqkv%d" % i, [128, 8, 512], BF16) for i in range(2)]
    stg = [E.sb("stg%d" % i, [128, 17 * 512], BF16) for i in range(2)]
    rC = [E.sb("ropeC%d" % i, [128, 128], F32) for i in range(2)]
    rS = [E.sb("ropeS%d" % i, [128, 128], F32) for i in range(2)]
    sets = []
    for i in range(2):
        o = i * 10240
        sets.append(dict(qn=scr(o, [512], F32), sq=scr(o + 2048, [512], F32), t1=scr(o + 4096, [512], F32),
                         t2=scr(o + 6144, [512], F32), qr=scr(o + 8192, [512], BF16), ss=scr(o + 9216, [4], F32),
                         rs=scr(o + 9248, [4], F32)))
    wsrc = d["w_qkv"].rearrange("(c p) j -> p c j", p=128)
    it = 0
    for k, cb in enumerate([4, 5, 6, 7, 0, 1, 2, 3]):
        w = wb[k % 2]
        P.dma("pool", w, wsrc[:, :, cb * 512:(cb + 1) * 512])
        sg = stg[k % 2]
        def stage1(t, S):
            ps = E.pa[t % 2]
            for c in range(8):
                MM(P, ps, E.AT[:, c, t * 128:(t + 1) * 128], w[:, c, :], c == 0, c == 7)
            if cb >= 6:
                COPY(P, "act", sg.rearrange("p (t c) -> p t c", t=17)[:, t, :], ps)
                return
            emit_headnorm(E, S["qn"], ps, 4, 128, gq if cb < 4 else gk, (S["sq"], S["ss"], S["rs"]))
            P.dma("sp", rC[t % 2], d["ropeC"][t])
            P.dma("sp", rS[t % 2], d["ropeS"][t])

        def stage2(t, S):
            if cb >= 6:
                return
            emit_rope(E, S["qr"], S["qn"], rC[t % 2], rS[t % 2], 4, 32, S["t1"], S["t2"])
            pt = E.pb[t % 2].bitcast(BF16)
            for h in range(4):
                TR(P, pt[:, h * 128:(h + 1) * 128], S["qr"][:, h * 128:(h + 1) * 128], identb)
            COPY(P, "act", sg.rearrange("p (h t) -> p h t", h=4)[:, :, t * 128:(t + 1) * 128], pt[:, 0:512].rearrange("p (h t) -> p h t", h=4))

        prev = None
        for t in range(NT):
            S = sets[it % 2]
            it += 1
            stage1(t, S)
            if prev is not None:
                stage2(*prev)
            prev = (t, S)
        stage2(*prev)
        if cb < 4:
            P.dma("sp", d["qT"][4 * cb:4 * cb + 4].rearrange("h p t -> p h t"), sg.rearrange("p (h t) -> p h t", h=4))
        elif cb < 6:
            P.dma("sp", d["kT"][4 * (cb - 4):4 * (cb - 4) + 4].rearrange("h p t -> p h t"), sg.rearrange("p (h t) -> p h t", h=4))
        else:
            P.dma("sp", d["v"].rearrange("(t p) c -> p t c", p=128)[:, :, (cb - 6) * 512:(cb - 5) * 512], sg.rearrange("p (t c) -> p t c", t=17))
        if cb == 7 and "gather" in d:
            d["gather"]()


def phase_attn(E, d, Hk, G, has_pe, scale, scr_t):
    P = E.P
    H = Hk * G
    KT = [E.sb("KT%d" % i, [128, NKEY], BF16) for i in range(2)]
    VG = [E.sb("VG%d" % i, [128, NKT, 128], BF16) for i in range(2)]
    QT = [E.sb("QT%d" % i, [128, NTOK], BF16) for i in range(2)]
    OTs = [E.sb("OTs%d" % i, [128, NTOK], BF16) for i in range(2)]
    PT = [E.sb("PT%d" % i, [128, 512], BF16) for i in range(3)] + [aview(scr_t, 16384 + i * 1024, [512], BF16) for i in range(2)]
    NPT = len(PT)
    accA = [aview(scr_t, i * 2048, [512], F32) for i in range(2)]
    accB = [aview(scr_t, 4096 + i * 2048, [512], F32) for i in range(2)]
    rl = [E.sb("rl%d" % i, [128, 512], F32) for i in range(2)]
    ones_b = E.sb("ones_b", [128, 128], BF16)
    MEMSET(P, "dve", ones_b, 1.0)
    if has_pe:
        KPE = E.sb("KPE", [128, NKEY], BF16)
        P.dma("sp", KPE.rearrange("p (r t) -> p r t", r=2), d["kpeT_all"])
        QPE = [E.sb("QPE%d" % i, [128, NTOK], BF16) for i in range(2)]
    ptc = [0]
    cc = [0]
    for g in range(Hk):
        kt_ = KT[g % 2]
        vg = VG[g % 2]
        P.dma("sp", kt_.rearrange("p (r t) -> p r t", r=2), d["kT_all"][g])
        for (t0, n, vgc) in d["v_chunks"]:
            for r in range(2):
                k0 = r * NT + t0 // 128
                P.dma("sp", vg[:, k0:k0 + n // 128, :], vgc[r * n:(r + 1) * n, g * 128:(g + 1) * 128].rearrange("(t p) c -> p t c", p=128))
        for gi in range(G):
            h = g * G + gi
            qt = QT[h % 2]
            ot = OTs[h % 2]
            P.dma("sp", qt, d["qT"][h])
            if has_pe:
                pb_ = (h % 2) * 64
                if h % 2 == 0:
                    qpe = QPE[(h // 2) % 2]
                    P.dma("sp", qpe, d["qpeT"][h // 2])
            for ci, (t0, n) in enumerate(CHUNKS):
                kts = list(range(NKT)) if ci < 4 else [16, 33]
                po = E.pb[cc[0] % 2]
                pl = E.py[cc[0] % 2]

                def qk(kt):
                    ps = E.pa[kt % 2] if ci < 4 else E.pa[kts.index(kt) % 2]
                    MM(P, ps[:, 0:n], kt_[:, kt * 128:(kt + 1) * 128], qt[:, t0:t0 + n], True, not has_pe)
                    if has_pe:
                        MM(P, ps[:, 0:n], KPE[pb_:pb_ + 64, kt * 128:(kt + 1) * 128], qpe[pb_:pb_ + 64, t0:t0 + n], False, True)
                    pt_ = PT[ptc[0] % NPT]
                    ptc[0] += 1
                    ACT(P, pt_[:, 0:n], ps[:, 0:n], AF.Exp, scale=scale)
                    return pt_

                def pv(kt, pt_, first, last, idx):
                    MM(P, po[:, 0:n], vg[:, kt, :], pt_[:, 0:n], first, last)
                    MM(P, pl[:, 0:n], ones_b, pt_[:, 0:n], first, last)

                prev = None
                for i, kt in enumerate(kts):
                    cur = (kt, qk(kt))
                    if prev is not None:
                        pv(prev[0], prev[1], i == 1, False, i - 1)
                    prev = cur
                pv(prev[0], prev[1], len(kts) == 1, True, len(kts) - 1)
                cc[0] += 1
                r = rl[ci % 2]
                P.op("dve", lambda e, r=r, pl=pl, n=n: e.reciprocal(out=r[:, 0:n], in_=pl[:, 0:n]), reads=[pl[:, 0:n]], writes=[r[:, 0:n]])
                TT(P, "dve", ot[:, t0:t0 + n], po[:, 0:n], r[:, 0:n], ALU.mult)
            P.dma("sp", d["oT"][h], ot)


def phase_oproj(E, d, H, g1row):
    P = E.P
    wo = E.sb("wo", [128, H, 1024], BF16)
    P.dma("pool", wo, d["w_o"].rearrange("(h p) j -> p h j", p=128))
    otl = [E.sb("otl%d" % i, [128, H, 128], BF16) for i in range(2)]
    tmp = E.sb("tmpo", [128, 1024], F32)
    osrc = d["oT"].rearrange("h p t -> p h t")
    for t in range(NT):
        o = otl[t % 2]
        P.dma("sp", o, osrc[:, :, t * 128:(t + 1) * 128])
        py = E.py[t % 2]
        for half in range(2):
            for h in range(H):
                MM(P, py[:, half * 512:(half + 1) * 512], o[:, h, :], wo[:, h, half * 512:(half + 1) * 512], h == 0, h == H - 1)
        emit_resid(E, t, py, g1row, tmp)


def phase_mla_proj(E, d):
    P = E.P
    E.AT = E.sb("AT", [128, 8, NTOK], BF16)
    SCR = E.sb("SCRm", [128, 10240], BF16)

    def scr(boff, shape, dt):
        return aview(SCR, boff, shape, dt)

    modT, gs1, _ = mix_prologue(E, d, SCR, want_g1row=False)
    emit_norm(E, std_tiles(E), gs1, modT[:, 0:8, :], scr)
    identb = E.sb("identb", [128, 128], BF16)
    COPY(P, "dve", identb, E.ident)
    gq = E.sb("gqm", [128, 512], F32)
    gkv = E.sb("gkvm", [128, 256], F32)
    P.dma("sp", gq, d["gq"])
    P.dma("sp", gkv, d["gkv"])
    wdkv = E.sb("wdkv", [128, 8, 320], BF16)
    wdq = E.sb("wdq", [128, 8, 512], BF16)
    wuqr = E.sb("wuqr", [128, 4, 512], BF16)
    wuqn = E.sb("wuqn", [128, 4, 1024], BF16)
    wukk = E.sb("wukk", [128, 2, 1024], BF16)
    wukv = E.sb("wukv", [128, 2, 1024], BF16)
    P.dma("pool", wdkv, d["w_dkv"].rearrange("(c p) j -> p c j", p=128))
    P.dma("pool", wdq, d["w_dq"].rearrange("(c p) j -> p c j", p=128))
    P.dma("pool", wuqr, d["w_uq_rope"].rearrange("(c p) j -> p c j", p=128))
    P.dma("pool", wuqn, d["w_uq_nope"].rearrange("(c p) j -> p c j", p=128))
    P.dma("pool", wukk, d["w_ukv_k"].rearrange("(c p) j -> p c j", p=128))
    P.dma("pool", wukv, d["w_ukv_v"].rearrange("(c p) j -> p c j", p=128))
    ckvT = E.sb("ckvT", [128, 2, NTOK], BF16)
    cqT = E.sb("cqT", [128, 4, NTOK], BF16)
    rC = [E.sb("ropeCm%d" % i, [128, 64], F32) for i in range(2)]
    rS = [E.sb("ropeSm%d" % i, [128, 64], F32) for i in range(2)]
    qpes = [E.sb("qpes%d" % i, [128, 4, 128], BF16) for i in range(2)]
    kpes = [E.sb("kpes%d" % i, [128, 128], BF16) for i in range(2)]
    stg = [E.sb("stgm%d" % i, [128, NTOK], BF16) for i in range(2)]
    vst = [E.sb("vst%d" % i, [128, 1024], BF16) for i in range(2)]
    kvs, qss = [], []
    for i in range(2):
        o = i * 10240
        kvs.append(dict(cn=scr(o, [256], F32), sq=scr(o + 1024, [256], F32), kpf=scr(o + 2048, [64], F32),
                        kt1=scr(o + 2304, [64], F32), kt2=scr(o + 2560, [64], F32), kpb=scr(o + 2816, [2, 64], BF16),
                        cb16=scr(o + 3072, [256], BF16), ss=scr(o + 3584, [1], F32), rs=scr(o + 3616, [1], F32)))
        qss.append(dict(qn=scr(o, [512], F32), sq=scr(o + 2048, [512], F32), qb16=scr(o + 4096, [512], BF16),
                        qpf=scr(o + 5120, [512], F32), qt1=scr(o + 2048, [512], F32), qt2=scr(o + 7168, [512], F32),
                        qpb=scr(o + 9216, [512], BF16), ss=E.sb("qss%d" % i, [128, 1], F32), rs=E.sb("qrs%d" % i, [128, 1], F32)))
    def kv1(t):
        S = kvs[t % 2]
        tc_ = slice(t * 128, (t + 1) * 128)
        pkv = E.pa[t % 2]
        for c in range(8):
            MM(P, pkv[:, 0:320], E.AT[:, c, tc_], wdkv[:, c, :], c == 0, c == 7)
        P.dma("sp", rC[t % 2], d["ropeC"][t])
        P.dma("sp", rS[t % 2], d["ropeS"][t])
        ACT(P, S["kpf"], pkv[:, 256:320], AF.Copy)
        emit_headnorm(E, S["cn"], pkv[:, 0:256], 1, 256, gkv, (S["sq"], S["ss"], S["rs"]))
        COPY(P, "dve", S["cb16"], S["cn"])

    def kv2(t):
        S = kvs[t % 2]
        tc_ = slice(t * 128, (t + 1) * 128)
        ptr = E.pb[t % 2].bitcast(BF16)
        emit_rope(E, S["kpb"][:, 0, :], S["kpf"], rC[t % 2], rS[t % 2], 1, 16, S["kt1"], S["kt2"])
        COPY(P, "dve", S["kpb"][:, 1, :], S["kpb"][:, 0, :])
        for kc in range(2):
            TR(P, ptr[:, kc * 128:(kc + 1) * 128], S["cb16"][:, kc * 128:(kc + 1) * 128], identb)
        TR(P, ptr[:, 256:384], S["kpb"].rearrange("p a b -> p (a b)"), identb)
        COPY(P, "act", ckvT[:, :, tc_], ptr[:, 0:256].rearrange("p (a b) -> p a b", a=2))
        kp = kpes[t % 2]
        COPY(P, "act", kp, ptr[:, 256:384])
        P.dma("sp", d["kpeT"][:, tc_], kp)
        pv_ = E.py[t % 2]
        for half in range(2):
            for kc in range(2):
                MM(P, pv_[:, half * 512:(half + 1) * 512], ckvT[:, kc, tc_], wukv[:, kc, half * 512:(half + 1) * 512], kc == 0, kc == 1)
        vs = vst[t % 2]
        COPY(P, "act", vs, pv_)
        P.dma("sp", d["v"][t * 128:(t + 1) * 128, :], vs)

    for t in range(NT):
        kv1(t)
        if t > 0:
            kv2(t - 1)
    kv2(NT - 1)
    k = 0

    def upproj(h, which):
        nonlocal k
        sg = stg[k % 2]
        k += 1
        for ci, (t0, n) in enumerate(CHUNKS):
            ps = E.pa[ci % 2]
            if which == 0:
                for kc in range(2):
                    MM(P, ps[:, 0:n], wukk[:, kc, h * 128:(h + 1) * 128], ckvT[:, kc, t0:t0 + n], kc == 0, kc == 1)
            else:
                for qc in range(4):
                    MM(P, ps[:, 0:n], wuqn[:, qc, h * 128:(h + 1) * 128], cqT[:, qc, t0:t0 + n], qc == 0, qc == 3)
            COPY(P, "act" if ci % 2 == 0 else "dve", sg[:, t0:t0 + n], ps[:, 0:n])
        P.dma("sp", (d["kT"] if which == 0 else d["qT"])[h], sg)

    for h in range(8):
        upproj(h, 0)
    if "gather" in d:
        d["gather"]()
    def q1(t):
        S = qss[t % 2]
        tc_ = slice(t * 128, (t + 1) * 128)
        pq = E.pb[t % 2]
        ptr = E.py[t % 2].bitcast(BF16)
        for c in range(8):
            MM(P, pq, E.AT[:, c, tc_], wdq[:, c, :], c == 0, c == 7)
        emit_headnorm(E, S["qn"], pq, 1, 512, gq, (S["sq"], S["ss"], S["rs"]))
        COPY(P, "dve", S["qb16"], S["qn"])
        for qc in range(4):
            TR(P, ptr[:, qc * 128:(qc + 1) * 128], S["qb16"][:, qc * 128:(qc + 1) * 128], identb)
        COPY(P, "act", cqT[:, :, tc_], ptr[:, 0:512].rearrange("p (a b) -> p a b", a=4))
        pqp = E.pa[t % 2]
        for qc in range(4):
            MM(P, pqp, cqT[:, qc, tc_], wuqr[:, qc, :], qc == 0, qc == 3)
        ACT(P, S["qpf"], pqp, AF.Copy)
        P.dma("sp", rC[t % 2], d["ropeC"][t])
        P.dma("sp", rS[t % 2], d["ropeS"][t])

    def q2(t):
        S = qss[t % 2]
        tc_ = slice(t * 128, (t + 1) * 128)
        ptr = E.py[t % 2].bitcast(BF16)
        emit_rope(E, S["qpb"], S["qpf"], rC[t % 2], rS[t % 2], 8, 16, S["qt1"], S["qt2"])
        for pr in range(4):
            TR(P, ptr[:, 1024 + pr * 128:1024 + (pr + 1) * 128], S["qpb"][:, pr * 128:(pr + 1) * 128], identb)
        qs = qpes[t % 2]
        COPY(P, "act", qs, ptr[:, 1024:1536].rearrange("p (a b) -> p a b", a=4))
        P.dma("sp", d["qpeT"].rearrange("h p t -> p h t")[:, :, tc_], qs)

    for t in range(NT):
        q1(t)
        if t > 0:
            q2(t - 1)
    q2(NT - 1)
    for h in range(8):
        upproj(h, 1)


def phase_attnB(E, d, Hk, G, has_pe, scale):
    SCR = E.sb("SCRb", [128, 10240], BF16)
    modT, gs1, g1row = mix_prologue(E, d, SCR)
    phase_attn(E, d, Hk, G, has_pe, scale, SCR)
    phase_oproj(E, d, Hk * G, g1row)


def phase_final(E, d):
    P = E.P
    gfin = E.sb("gfin", [128, 1024], F32)
    P.dma("sp", gfin, d["gfin"])
    junk = E.sb("junkf", [128, 1024], BF16)
    ss = E.sb("ssf", [128, 16], F32)
    sq = E.sb("sqf", [128, 16], F32)
    for t in range(16):
        ACT(P, junk, E.X[:, t, :], AF.Square, accum_out=ss[:, t:t + 1])
    TS(P, "dve", ss, ss, 1.0 / D, ALU.mult, EPS, ALU.add)
    ACT(P, sq, ss, AF.Sqrt)
    P.op("dve", lambda e: e.reciprocal(out=ss, in_=sq), reads=[sq], writes=[ss])
    for t in range(16):
        TS(P, "dve", E.X[:, t, :], E.X[:, t, :], ss[:, t:t + 1], ALU.mult)
        TT(P, "pool", E.X[:, t, :], E.X[:, t, :], gfin, ALU.mult)


MLA_SCALE = 192 ** -0.5
GQA_SCALE = 128 ** -0.5


def _mixd(E, lay):
    return {"w_mod": E.din("w_mod_%d" % lay, [D, 6 * D]), "bmodT": E.din("bmodT_%d" % lay, [128, 48]),
            "gmix": E.din("gmix_%d" % lay, [128, 8])}


def _kv_io(E, lay, fused, H, Hk, has_pe):
    mk = E.dscr if fused else E.dout
    io = {"qT": mk("qT_%d" % lay, [H, 128, NTOK], BF16), "kT": mk("kT_%d" % lay, [Hk, 128, NTOK], BF16),
          "v": mk("v_%d" % lay, [NTOK, Hk * 128], BF16)}
    if has_pe:
        io["qpeT"] = mk("qpeT_%d" % lay, [H // 2, 128, NTOK], BF16)
        io["kpeT"] = mk("kpeT_%d" % lay, [128, NTOK], BF16)
    return io


def _make_gather(E, lay, io, Hk, has_pe):
    def gather():
        kTg = []
        for g in range(Hk):
            kg = E.dscr("kTg_%d_%d" % (lay, g), [256, NTOK], BF16)
            ALLGATHER(E.P, kg, io["kT"][g])
            kTg.append(kg.rearrange("(r p) t -> p r t", r=2))
        vch = []
        for ci, (t0, n) in enumerate(CHUNKS):
            vgc = E.dscr("vg_%d_%d" % (lay, ci), [2 * n, Hk * 128], BF16)
            ALLGATHER(E.P, vgc, io["v"][t0:t0 + n, :])
            vch.append((t0, n, vgc))
        kv = {"kT_all": kTg, "v_chunks": vch}
        if has_pe:
            kpeg = E.dscr("kpeg_%d" % lay, [256, NTOK], BF16)
            ALLGATHER(E.P, kpeg, io["kpeT"])
            kv["kpeT_all"] = kpeg.rearrange("(r p) t -> p r t", r=2)
        if not hasattr(E, "kv"):
            E.kv = {}
        E.kv[lay] = kv
    return gather


def emit_phase(E, ph):
    kind = ph[0]
    if kind == "moe":
        lay = ph[1]
        d = {"w_mod": E.din("w_mod_%d" % lay, [D, 6 * D]), "bmodT": E.din("bmodT_%d" % lay, [128, 48]),
             "gffn": E.din("gffn_%d" % lay, [128, 8]), "wr": E.din("wr_%d" % lay, [128, 8, 36]),
             "bg": E.din("bg_%d" % lay, [128, 4]), "be": E.din("be_%d" % lay, [128, 32]),
             "w_gate": E.din("w_gate_%d" % lay, [32, D, 512]), "w_up": E.din("w_up_%d" % lay, [32, D, 512]),
             "w_down": E.din("w_down_%d" % lay, [32, 512, D])}
        phase_moe(E, d, chunks=CHUNKS[:4] if lay == 3 else None)
    elif kind == "conv":
        lay = ph[1]
        P = E.P
        hsrc = E.dscr("hsrc_%d" % lay, [4, D])
        hgat = E.dscr("hgat_%d" % lay, [8, D])
        P.dma("sp", hsrc[0:1, :], E.X[0:1, 0, :])
        P.dma("sp", hsrc[1:2, :], E.X[127:128, 15, :])
        P.dma("sp", hsrc[2:3, :], E.X[0:1, 16, :])
        P.dma("sp", hsrc[3:4, :], E.X[127:128, 16, :])
        ALLGATHER(P, hgat, hsrc)
        d = _mixd(E, lay)
        d.update({"cw": E.din("cw_%d" % lay, [128, 8, 3]), "hmask": E.din("hmask", [128, 4]),
                  "x_halo_rows": [hgat[1:2, :], hgat[4:5, :], hgat[3:4, :], hgat[6:7, :]],
                  "w_in": E.din("w_in_%d" % lay, [D, 3 * D]), "w_out": E.din("w_out_%d" % lay, [D, D])})
        phase_conv(E, d)
    elif kind == "gqaA":
        lay = ph[1]
        d = _mixd(E, lay)
        d.update({"gq": E.din("gq_%d" % lay, [128, 128]), "gk": E.din("gk_%d" % lay, [128, 128]),
                  "ropeC": E.din("ropeC32", [NT, 128, 128]), "ropeS": E.din("ropeS32", [NT, 128, 128]),
                  "w_qkv": E.din("w_qkv_%d" % lay, [D, 4096])})
        io = _kv_io(E, lay, True, 16, 8, False)
        d.update(io)
        d["gather"] = _make_gather(E, lay, io, 8, False)
        phase_gqa_proj(E, d)
    elif kind == "mlaA":
        lay = ph[1]
        d = _mixd(E, lay)
        d.update({"gq": E.din("gq_%d" % lay, [128, 512]), "gkv": E.din("gkv_%d" % lay, [128, 256]),
                  "ropeC": E.din("ropeC16", [NT, 128, 64]), "ropeS": E.din("ropeS16", [NT, 128, 64]),
                  "w_dkv": E.din("w_dkv_%d" % lay, [D, 320]), "w_dq": E.din("w_dq_%d" % lay, [D, 512]),
                  "w_uq_rope": E.din("w_uq_rope_%d" % lay, [512, 512]), "w_uq_nope": E.din("w_uq_nope_%d" % lay, [512, 1024]),
                  "w_ukv_k": E.din("w_ukv_k_%d" % lay, [256, 1024]), "w_ukv_v": E.din("w_ukv_v_%d" % lay, [256, 1024])})
        io = _kv_io(E, lay, True, 8, 8, True)
        d.update(io)
        d["gather"] = _make_gather(E, lay, io, 8, True)
        phase_mla_proj(E, d)
    elif kind in ("gqaB", "mlaB"):
        lay = ph[1]
        Hk, G, has_pe, scale = (8, 2, False, GQA_SCALE) if kind == "gqaB" else (8, 1, True, MLA_SCALE)
        H = Hk * G
        d = _mixd(E, lay)
        io = _kv_io(E, lay, True, H, Hk, has_pe)
        d.update(E.kv[lay])
        d.update({"qT": io["qT"], "w_o": E.din("w_o_%d" % lay, [H * 128, D]), "oT": E.dscr("oT_%d" % lay, [H, 128, NTOK], BF16)})
        if has_pe:
            d["qpeT"] = io["qpeT"]
        phase_attnB(E, d, Hk, G, has_pe, scale)
    elif kind == "final":
        phase_final(E, {"gfin": E.din("gfin", [128, 1024])})
    else:
        raise ValueError(kind)


_PROG_CACHE = {}


def build_program(phases):
    key = tuple(phases)
    if key in _PROG_CACHE:
        return _PROG_CACHE[key]
    nc = bass.Bass("TRN2", target_bir_lowering=False)
    st = ExitStack()
    with st:
        E = Env(nc, st)
        setup_common(E)
        d_x = E.din("x_in", [NTOK, D])
        d_y = E.dout("x_out", [NTOK, D])
        load_x(E, d_x)
        for ph in phases:
            with E.phase():
                emit_phase(E, ph)
        store_x(E, d_y)
        E.P.barrier()
        E.P.emit()
    _PROG_CACHE[key] = nc
    return nc


def _fm(v):
    return np.ascontiguousarray(np.asarray(v, np.float32).reshape(-1, 128).T)


def _rows(v, n=128):
    v = np.asarray(v, np.float32)
    return np.ascontiguousarray(np.broadcast_to(v, (n,) + v.shape))


def _rope_tables(h, half):
    inv = (10000.0 ** (-np.arange(half, dtype=np.float32) / half)).astype(np.float32)
    C = np.ones((NT, 128, 2, 2, half), np.float32)
    S = np.zeros((NT, 128, 2, 2, half), np.float32)
    t = h * 2048 + np.arange(2048)
    for a, pos in enumerate((t // 64, t % 64)):
        ang = pos.astype(np.float32)[:, None] * inv[None, :]
        c = np.cos(ang).reshape(16, 128, half)
        s = np.sin(ang).reshape(16, 128, half)
        C[:16, :, a, 0] = c
        C[:16, :, a, 1] = c
        S[:16, :, a, 0] = -s
        S[:16, :, a, 1] = s
    return C.reshape(NT, 128, 4 * half), S.reshape(NT, 128, 4 * half)


def _layer_inputs(inp, ph):
    kind = ph[0]
    o = {}
    if kind == "final":
        o["gfin"] = _rows(inp["final_norm_g"])
        return o
    lay = ph[1]
    o["w_mod_%d" % lay] = inp["w_mod"][lay]
    o["bmodT_%d" % lay] = _fm(inp["b_mod"][lay])
    j = lay // 3
    if kind == "moe":
        wr = np.concatenate([inp["moe_w_group"][lay], inp["moe_w_expert"][lay]], 1)
        o["gffn_%d" % lay] = _fm(inp["norm_ffn_g"][lay])
        o["wr_%d" % lay] = np.ascontiguousarray(wr.reshape(8, 128, 36).transpose(1, 0, 2))
        o["bg_%d" % lay] = _rows(inp["moe_b_group"][lay])
        o["be_%d" % lay] = _rows(inp["moe_b_expert"][lay])
        o["w_gate_%d" % lay] = inp["moe_w_gate"][lay]
        o["w_up_%d" % lay] = inp["moe_w_up"][lay]
        o["w_down_%d" % lay] = inp["moe_w_down"][lay]
        return o
    o["gmix_%d" % lay] = _fm(inp["norm_mix_g"][lay])
    if kind == "conv":
        o["cw_%d" % lay] = np.ascontiguousarray(inp["conv_w"][j].reshape(3, 8, 128).transpose(2, 1, 0))
        o["w_in_%d" % lay] = inp["conv_w_in"][j]
        o["w_out_%d" % lay] = inp["conv_w_out"][j]
    elif kind == "gqaA":
        o["gq_%d" % lay] = _rows(inp["gqa_q_norm_g"][j])
        o["gk_%d" % lay] = _rows(inp["gqa_k_norm_g"][j])
        o["w_qkv_%d" % lay] = inp["gqa_w_qkv"][j]
    elif kind == "gqaB":
        o["w_o_%d" % lay] = inp["gqa_w_o"][j]
    elif kind == "mlaA":
        wuq = inp["mla_w_uq"][j].reshape(512, 8, 192)
        wukv = inp["mla_w_ukv"][j].reshape(256, 8, 256)
        o["gq_%d" % lay] = _rows(inp["mla_g_q"][j])
        o["gkv_%d" % lay] = _rows(inp["mla_g_kv"][j])
        o["w_dkv_%d" % lay] = inp["mla_w_dkv"][j]
        o["w_dq_%d" % lay] = inp["mla_w_dq"][j]
        o["w_uq_nope_%d" % lay] = np.ascontiguousarray(wuq[:, :, :128].reshape(512, 1024))
        o["w_uq_rope_%d" % lay] = np.ascontiguousarray(wuq[:, :, 128:].reshape(512, 512))
        o["w_ukv_k_%d" % lay] = np.ascontiguousarray(wukv[:, :, :128].reshape(256, 1024))
        o["w_ukv_v_%d" % lay] = np.ascontiguousarray(wukv[:, :, 128:].reshape(256, 1024))
    elif kind == "mlaB":
        o["w_o_%d" % lay] = inp["mla_w_o"][j]
    return o


def _core_tokens(xb, hb, h):
    return np.ascontiguousarray(np.concatenate([xb[h * 2048:(h + 1) * 2048], hb[h * 128:(h + 1) * 128]], 0))


def _run_group(phases, inp, xs, per_core_extra):
    nc = build_program(phases)
    shared = {"ident_in": np.eye(128, dtype=np.float32)}
    for ph in phases:
        shared.update(_layer_inputs(inp, ph))
    kinds = [p[0] for p in phases]
    in_maps = []
    for core in range(8):
        b, h = core // 2, core % 2
        m = dict(shared)
        m["cond_in"] = np.ascontiguousarray(np.stack([_fm(inp["c"][b]), _fm(inp["c_ctx"])], -1))
        m["x_in"] = xs[core]
        if "gqaA" in kinds:
            m["ropeC32"], m["ropeS32"] = _rope_tables(h, 32)
        if "mlaA" in kinds:
            m["ropeC16"], m["ropeS16"] = _rope_tables(h, 16)
        if "conv" in kinds:
            hm = np.zeros(4, np.float32)
            hm[[0, 2]] = 1.0 if h > 0 else 0.0
            hm[[1, 3]] = 1.0 if h < 1 else 0.0
            m["hmask"] = _rows(hm)
        m.update(per_core_extra[core])
        in_maps.append(m)
    res = run_bass_kernel_spmd(nc, in_maps, core_ids=list(range(8)))
    return res.results


ALL_PHASES = (("mlaA", 0), ("mlaB", 0), ("moe", 0), ("conv", 1), ("moe", 1), ("gqaA", 2), ("gqaB", 2), ("moe", 2),
              ("mlaA", 3), ("mlaB", 3), ("moe", 3), ("final",))


def kernel(**inputs):
    inp = {k: np.asarray(v) for k, v in inputs.items()}
    x, ctx = inp["x"].astype(np.float32, copy=False), inp["ctx"].astype(np.float32, copy=False)
    xs = [_core_tokens(x[c // 2], ctx[c // 2], c % 2) for c in range(8)]
    r = _run_group(ALL_PHASES, inp, xs, [{} for _ in range(8)])
    out = np.empty((4, 4096, D), np.float32)
    for c in range(8):
        out[c // 2, (c % 2) * 2048:(c % 2 + 1) * 2048] = r[c]["x_out"][:2048]
    return out
```

```python
import numpy as np
from contextlib import ExitStack
import concourse.bass as bass
import concourse.mybir as mybir
from concourse.bass_utils import run_bass_kernel_spmd

F32 = mybir.dt.float32
BF16 = mybir.dt.bfloat16
AF = mybir.ActivationFunctionType
ALU = mybir.AluOpType
AX = mybir.AxisListType

D = 1024
NT = 17
NTOK = NT * 128
CHUNKS = [(0, 512), (512, 512), (1024, 512), (1536, 512), (2048, 128)]
EPS = 1e-6
NKEY = 2 * NTOK
NKT = NKEY // 128


def _dsz(dt):
    if dt == F32:
        return 4
    if dt == BF16:
        return 2
    s = str(dt)
    if "32" in s:
        return 4
    if "16" in s:
        return 2
    if "64" in s:
        return 8
    return 1


def _box(ap):
    t = ap.tensor
    a = ap.ap
    dsz = _dsz(ap.dtype)
    off = int(ap.offset)
    tname = type(t).__name__
    if tname.startswith("DRam"):
        lo = off
        hi = off + sum((c - 1) * s for s, c in a) + 1
        return (t.name, 0, 1, lo * dsz, hi * dsz)
    pstep, pcnt = a[0]
    if pstep == 0:
        p0, f0 = 0, off
    else:
        p0 = off // pstep
        f0 = off - p0 * pstep
    hi = f0 + sum((c - 1) * s for s, c in a[1:]) + 1
    if tname.startswith("PSum"):
        b0 = (f0 * dsz) // 2048 * 2048
        b1 = -(-(hi * dsz) // 2048) * 2048
        return (t.name, 0, 128, b0, b1)
    return (t.name, p0, p0 + pcnt, f0 * dsz, hi * dsz)


class Prog:
    CE = ("pe", "act", "dve", "pool")
    ALLQ = ("pe", "act", "dve", "pool", "sp")

    def __init__(self, nc, stack, n_dma_sems=24):
        self.nc = nc
        self.ops = {e: [] for e in self.ALLQ}
        self.esem = {e: stack.enter_context(nc.semaphore("es_" + e)) for e in self.CE}
        self.ecnt = {e: 0 for e in self.CE}
        self.known = {e: {} for e in self.ALLQ}
        self.hist = {}
        self.dsem = {}
        self.dpos = {}
        for q in ("sp", "pool", "act", "cc"):
            n = n_dma_sems if q in ("sp", "pool") else 4
            self.dsem[q] = [[stack.enter_context(nc.semaphore("ds_%s%d" % (q, i))), 0] for i in range(n)]
            self.dpos[q] = 0
        self.nops = 0

    def _need(self, eng, tok, waits):
        key, sem, val, peng = tok
        if peng == eng and eng == "pe":
            return
        if self.known[eng].get(key, 0) >= val:
            return
        if waits.get(key, (None, 0))[1] < val:
            waits[key] = (sem, val)

    def _deps(self, eng, reads, writes):
        waits = {}
        rb = [_box(a) for a in reads]
        wb = [_box(a) for a in writes]
        for b in rb:
            for r in self.hist.get(b[0], ()):
                if r[4] == "w" and r[0] < b[2] and b[1] < r[1] and r[2] < b[4] and b[3] < r[3]:
                    self._need(eng, r[5], waits)
        for b in wb:
            for r in self.hist.get(b[0], ()):
                if r[0] < b[2] and b[1] < r[1] and r[2] < b[4] and b[3] < r[3]:
                    self._need(eng, r[5], waits)
        return waits, rb, wb

    def _record(self, eng, tok, rb, wb):
        for b in wb:
            h = self.hist.setdefault(b[0], [])
            h[:] = [r for r in h if not (b[1] <= r[0] and r[1] <= b[2] and b[3] <= r[2] and r[3] <= b[4])]
            h.append([b[1], b[2], b[3], b[4], "w", tok, eng])
        for b in rb:
            h = self.hist.setdefault(b[0], [])
            if tok[3] == eng:
                h[:] = [r for r in h if not (r[4] == "r" and r[5][3] == eng
                                             and b[1] <= r[0] and r[1] <= b[2] and b[3] <= r[2] and r[3] <= b[4])]
            h.append([b[1], b[2], b[3], b[4], "r", tok, eng])

    def op(self, eng, fn, reads=(), writes=()):
        waits, rb, wb = self._deps(eng, reads, writes)
        self.ecnt[eng] += 1
        tok = ("e_" + eng, self.esem[eng], self.ecnt[eng], eng)
        wl = list(waits.items())
        for key, (sem, val) in wl:
            self.known[eng][key] = val
        self.ops[eng].append(([w for _, w in wl], fn, self.esem[eng], 1))
        self._record(eng, tok, rb, wb)
        self.nops += 1
        return tok

    def dma(self, q, out, in_, **kw):
        waits, rb, wb = self._deps(q, [in_], [out])
        idx = self.dpos[q] % len(self.dsem[q])
        slot = self.dsem[q][idx]
        self.dpos[q] += 1
        key = "d_%s%d" % (q, idx)
        sem = slot[0]
        if slot[1] > 0:
            self._need(q, (key, sem, slot[1], "dma"), waits)
        slot[1] += 16
        tok = (key, sem, slot[1], "dma")
        wl = list(waits.items())
        for k, (s, v) in wl:
            self.known[q][k] = v

        def fn(e, out=out, in_=in_, kw=kw):
            return e.dma_start(out=out, in_=in_, **kw)

        self.ops[q].append(([w for _, w in wl], fn, sem, 16))
        self._record(q, tok, rb, wb)
        self.nops += 1
        return tok

    def custom_dma(self, q, fn, reads, writes, inc=16, pool=None):
        pool = pool or q
        waits, rb, wb = self._deps(q, reads, writes)
        idx = self.dpos[pool] % len(self.dsem[pool])
        slot = self.dsem[pool][idx]
        self.dpos[pool] += 1
        key = "d_%s%d" % (pool, idx)
        sem = slot[0]
        if slot[1] > 0:
            self._need(q, (key, sem, slot[1], "dma"), waits)
        slot[1] += inc
        tok = (key, sem, slot[1], "dma")
        wl = list(waits.items())
        for k, (s_, v) in wl:
            self.known[q][k] = v
        self.ops[q].append(([w for _, w in wl], fn, sem, inc))
        self._record(q, tok, rb, wb)
        self.nops += 1
        return tok

    def wait_tok(self, eng, tok):
        key, sem, val, peng = tok
        if self.known[eng].get(key, 0) >= val:
            return
        self.known[eng][key] = val
        self.ops[eng].append(([(sem, val)], None, None, 0))

    def barrier(self):
        toks = [("e_" + e, self.esem[e], self.ecnt[e], e) for e in self.CE if self.ecnt[e] > 0]
        for q in self.dsem:
            for i, slot in enumerate(self.dsem[q]):
                if slot[1] > 0:
                    toks.append(("d_%s%d" % (q, i), slot[0], slot[1], "dma"))
        for q in self.ALLQ:
            for tk in toks:
                if tk[3] == q:
                    continue
                self.wait_tok(q, tk)
        self.hist = {}

    def emit(self):
        nc = self.nc
        engmap = {"pe": "tensor", "act": "scalar", "dve": "vector", "pool": "gpsimd", "sp": "sync"}
        with nc.Block() as block:
            for q in self.ALLQ:
                lst = self.ops[q]
                if not lst:
                    continue

                def body(e, lst=lst):
                    for waits, fn, sem, inc in lst:
                        for s, v in waits:
                            e.wait_ge(s, v)
                        if fn is not None:
                            fn(e).then_inc(sem, inc)

                getattr(block, engmap[q])(body)


class Env:
    def __init__(self, nc, st):
        self.nc = nc
        self.st = st
        self.P = Prog(nc, st)
        self._n = 0
        self.cur = st
        self.dram = {}

    def sb(self, name, shape, dt):
        return self.cur.enter_context(self.nc.sbuf_tensor("sb%d_%s" % (self._n, name), list(shape), dt))[:]

    def phase(self):
        env = self

        class _Ph:
            def __enter__(self_):
                env._n += 1
                self_.prev = env.cur
                self_.stk = ExitStack()
                self_.stk.__enter__()
                env.cur = self_.stk
                return env

            def __exit__(self_, *a):
                env.P.barrier()
                env.cur = self_.prev
                self_.stk.__exit__(None, None, None)
                return False

        return _Ph()

    def ps(self, name, shape, dt=F32):
        return self.st.enter_context(self.nc.psum_tensor(name, list(shape), dt))[:]

    def din(self, name, shape, dt=F32):
        if name not in self.dram:
            self.dram[name] = self.nc.dram_tensor(name, list(shape), dt, kind="ExternalInput").ap()
        return self.dram[name]

    def dout(self, name, shape, dt=F32):
        if name not in self.dram:
            self.dram[name] = self.nc.dram_tensor(name, list(shape), dt, kind="ExternalOutput").ap()
        return self.dram[name]

    def dscr(self, name, shape, dt=F32):
        if name not in self.dram:
            self.dram[name] = self.nc.dram_tensor(name, list(shape), dt, kind="Internal").ap()
        return self.dram[name]


def aview(t, boff, shape, dt):
    n = int(np.prod(shape)) * _dsz(dt)
    assert boff % 4 == 0
    ap = t[:, boff // 2:(boff + n) // 2]
    if dt != BF16:
        ap = ap.bitcast(dt)
    if len(shape) == 2:
        ap = ap.rearrange("p (a b) -> p a b", a=shape[0])
    elif len(shape) == 3:
        ap = ap.rearrange("p (a b c) -> p a b c", a=shape[0], b=shape[1])
    return ap


PAIRS = [[0, 1], [2, 3], [4, 5], [6, 7]]


def ALLGATHER(P, out, in_):
    P.custom_dma("pool", lambda e: e.collective_compute("AllGather", ALU.bypass, replica_groups=PAIRS, ins=[in_], outs=[out]),
                 reads=[in_], writes=[out], inc=1, pool="cc")


def bc(ap, shape):
    return ap.broadcast_to(list(shape))


def TT(P, eng, out, in0, in1, op):
    P.op(eng, lambda e: e.tensor_tensor(out=out, in0=in0, in1=in1, op=op), reads=[in0, in1], writes=[out])


def TS(P, eng, out, in0, s1, op0, s2=None, op1=None):
    rd = [in0] + [s for s in (s1, s2) if not isinstance(s, (int, float, type(None)))]
    if op1 is None:
        P.op(eng, lambda e: e.tensor_scalar(out=out, in0=in0, scalar1=s1, scalar2=None, op0=op0), reads=rd, writes=[out])
    else:
        P.op(eng, lambda e: e.tensor_scalar(out=out, in0=in0, scalar1=s1, scalar2=s2, op0=op0, op1=op1), reads=rd, writes=[out])


def STT(P, eng, out, in0, scalar, in1, op0, op1):
    rd = [in0, in1] + ([] if isinstance(scalar, (int, float)) else [scalar])
    P.op(eng, lambda e: e.scalar_tensor_tensor(out=out, in0=in0, scalar=scalar, in1=in1, op0=op0, op1=op1), reads=rd, writes=[out])


def RED(P, eng, out, in_, op):
    P.op(eng, lambda e: e.tensor_reduce(out=out, in_=in_, axis=AX.X, op=op), reads=[in_], writes=[out])


def ACT(P, out, in_, func, bias=None, scale=None, accum_out=None):
    rd = [in_]
    kw = {}
    if bias is not None:
        kw["bias"] = bias
        if not isinstance(bias, (int, float)):
            rd.append(bias)
    if scale is not None:
        kw["scale"] = scale
        if not isinstance(scale, (int, float)):
            rd.append(scale)
    wr = [out]
    if accum_out is not None:
        kw["accum_out"] = accum_out
        wr.append(accum_out)
    P.op("act", lambda e: e.activation(out=out, in_=in_, func=func, **kw), reads=rd, writes=wr)


def COPY(P, eng, out, in_):
    if eng == "act":
        P.op("act", lambda e: e.copy(out=out, in_=in_), reads=[in_], writes=[out])
    else:
        P.op(eng, lambda e: e.tensor_copy(out=out, in_=in_), reads=[in_], writes=[out])


def MM(P, out, lhsT, rhs, start, stop):
    P.op("pe", lambda e: e.matmul(out, lhsT=lhsT, rhs=rhs, start=start, stop=stop), reads=[lhsT, rhs], writes=[out])


def TR(P, out, in_, ident):
    P.op("pe", lambda e: e.transpose(out=out, in_=in_, identity=ident), reads=[in_, ident], writes=[out])


def MEMSET(P, eng, ap, val):
    P.op(eng, lambda e: e.memset(ap, val), writes=[ap])


def setup_common(E):
    P = E.P
    E.X = E.sb("X", [128, NT, D], F32)
    E.ident = E.sb("ident", [128, 128], F32)
    E.ones_f = E.sb("ones_f", [128, 128], F32)
    E.cond = E.sb("cond", [128, 8, 2], F32)
    E.scond = E.sb("scond", [128, 8, 2], F32)
    E.pa = [E.ps("pa%d" % i, [128, 512]) for i in range(2)]
    E.pb = [E.ps("pb%d" % i, [128, 512]) for i in range(2)]
    E.py = [E.ps("py%d" % i, [128, 1024]) for i in range(2)]
    E.d_ident = E.din("ident_in", [128, 128])
    E.d_cond = E.din("cond_in", [128, 8, 2])
    P.dma("sp", E.ident, E.d_ident)
    P.dma("sp", E.cond, E.d_cond)
    MEMSET(P, "dve", E.ones_f, 1.0)
    ACT(P, E.scond, E.cond, AF.Silu)
    E.modAll = E.sb("modAll", [128, 4, 48, 2], F32)
    E.bmodAll = E.sb("bmodAll", [128, 4, 48], F32)
    E.mod_done = set()
    E.bg = []
    for lay in getattr(E, "lays", ()):
        P.dma("sp", E.bmodAll[:, lay, :], E.din("bmodT_%d" % lay, [128, 48]))


def load_x(E, d_x):
    for t in range(NT):
        E.P.dma("sp", E.X[:, t, :], d_x[t * 128:(t + 1) * 128, :])


def store_x(E, d_y):
    toks = []
    for t in range(NT):
        toks.append(E.P.dma("sp", d_y[t * 128:(t + 1) * 128, :], E.X[:, t, :]))
    for tk in toks:
        E.P.wait_tok("sp", tk)


def emit_mod(E, d_wmod, bmodT, jc0, njc, wm, modT):
    P = E.P
    wsrc = d_wmod.rearrange("(c p) j -> p c j", p=128)
    ps = E.pa[0]
    for b in range(njc // 4):
        P.dma("sp", wm, wsrc[:, :, (jc0 + 4 * b) * 128:(jc0 + 4 * b + 4) * 128])
        for jj in range(4):
            j = 4 * b + jj
            for c in range(8):
                MM(P, ps[:, 2 * j:2 * j + 2], wm[:, c, jj * 128:(jj + 1) * 128], E.scond[:, c, :], c == 0, c == 7)
    for j in range(njc):
        TS(P, "dve", modT[:, j, :], ps[:, 2 * j:2 * j + 2], bmodT[:, jc0 + j:jc0 + j + 1], ALU.add)


def get_mod(E, lay, d_wmod, jc0, njc, wm):
    need = [(lay, j) for j in range(jc0, jc0 + njc) if (lay, j) not in E.mod_done]
    if need:
        emit_mod(E, d_wmod, E.bmodAll[:, lay, :], jc0, njc, wm, E.modAll[:, lay, jc0:jc0 + njc, :])
        E.mod_done.update((lay, j) for j in range(jc0, jc0 + njc))
    return E.modAll[:, lay, jc0:jc0 + njc, :]


def make_mod_jobs(E, sched, stage):
    P = E.P
    blocks = []
    for (lay, jc0, njc) in sched:
        for j in range(jc0, jc0 + njc, 2):
            if (lay, j) not in E.mod_done:
                blocks.append((lay, j))
                E.mod_done.update([(lay, j), (lay, j + 1)])

    def dma(k):
        lay, j = blocks[k]
        src = E.din("w_mod_%d" % lay, [D, 6 * D]).rearrange("(c p) j -> p c j", p=128)
        P.dma("pool", stage[k % 2], src[:, :, j * 128:(j + 2) * 128])

    def job(k):
        def run():
            if k == 0:
                dma(0)
            if k + 1 < len(blocks):
                dma(k + 1)
            lay, j = blocks[k]
            ps = E.py[k % 2][:, 512:516]
            wmk = stage[k % 2]
            for jj in range(2):
                for c in range(8):
                    MM(P, ps[:, 2 * jj:2 * jj + 2], wmk[:, c, jj * 128:(jj + 1) * 128], E.scond[:, c, :], c == 0, c == 7)
            for jj in range(2):
                TS(P, "dve", E.modAll[:, lay, j + jj, :], ps[:, 2 * jj:2 * jj + 2], E.bmodAll[:, lay, j + jj:j + jj + 1], ALU.add)
        return run

    return [job(k) for k in range(len(blocks))]


def emit_rowbc(E, vec, row, diag):
    P = E.P
    ps = E.py[0]
    for c in range(8):
        dg = diag[:, c % 2, :]
        TS(P, "dve", dg, E.ident, vec[:, c:c + 1], ALU.mult)
        MM(P, ps[:, c * 128:(c + 1) * 128], E.ones_f, dg, True, True)
    COPY(P, "act", row, ps[:])


def std_tiles(E, LG=None):
    tl = []
    for t in range(NT):
        n = 0 if t < 16 else 1
        tl.append(dict(src=E.X[:, t, :], dst=E.AT[:, :, t * 128:(t + 1) * 128], groups=[(0, 128, n)],
                       lg=None if LG is None else LG[:, t, :]))
    return tl


def emit_norm(E, tiles, gs, sh, scr, Wr=None):
    P = E.P
    ntl = len(tiles)
    junk = scr(0, [1024], BF16)
    ss = scr(2048, [ntl], F32)
    ms = scr(2048 + 128, [ntl], F32)
    sq = scr(2048 + 256, [ntl], F32)
    rstd = scr(2048 + 384, [ntl], F32)
    xn = [scr(4096 + i * 4096, [1024], F32) for i in range(2)]
    ft = [scr(12288 + i * 4096, [8, 128], F32) for i in range(2)]
    for t, tl in enumerate(tiles):
        ACT(P, junk, tl["src"], AF.Square, accum_out=ss[:, t:t + 1])
    TS(P, "dve", ms, ss, 1.0 / D, ALU.mult, EPS, ALU.add)
    ACT(P, sq, ms, AF.Sqrt)
    P.op("dve", lambda e: e.reciprocal(out=rstd, in_=sq), reads=[sq], writes=[rstd])
    def st1(t, tl):
        x_n = xn[t % 2]
        TS(P, "dve", x_n, tl["src"], rstd[:, t:t + 1], ALU.mult)
        ps = E.py[t % 2]
        for c in range(8):
            TR(P, ps[:, c * 128:(c + 1) * 128], x_n[:, c * 128:(c + 1) * 128], E.ident)

    def st2(t, tl):
        f_t = ft[t % 2]
        ps = E.py[t % 2]
        ncol = 0
        for (c0, c1, n) in tl["groups"]:
            ncol = max(ncol, c1)
            for c in range(8):
                ACT(P, f_t[:, c, c0:c1], ps[:, c * 128 + c0:c * 128 + c1], AF.Identity, bias=sh[:, c, n:n + 1], scale=gs[:, c, n:n + 1])
        COPY(P, "pool", tl["dst"], f_t[:, :, 0:ncol])
        if tl.get("lg") is not None:
            pl = E.pa[t % 2]
            for c in range(8):
                MM(P, pl[:, 0:36], f_t[:, c, :], Wr[:, c, :], c == 0, c == 7)
            COPY(P, "dve", tl["lg"], pl[:, 0:36])

    for t, tl in enumerate(tiles):
        st1(t, tl)
        if t > 0:
            st2(t - 1, tiles[t - 1])
    st2(len(tiles) - 1, tiles[-1])


def emit_routing(E, LG, bg, be, C, scr):
    P = E.P
    T = NT
    o = [0]

    def al(shape):
        n = int(np.prod(shape)) * 4
        a = scr(o[0], shape, F32)
        o[0] += (n + 31) // 32 * 32
        return a

    gl = LG[:, :, 0:4]
    el4 = LG[:, :, 4:36].rearrange("p t (g e) -> p t g e", g=4)
    v1 = al([T]); v2 = al([T]); v3 = al([T])
    g4a = al([T, 4]); g4b = al([T, 4]); goh = al([T, 4])
    p48 = al([T, 4, 8])
    e8a = al([T, 8]); e8b = al([T, 8]); ep = al([T, 8]); oh1 = al([T, 8]); oh2 = al([T, 8])
    gw = al([T])
    assert o[0] <= 20480, o[0]

    def b3(v, k):
        return bc(v.unsqueeze(2), [128, T, k])

    RED(P, "dve", v1, gl, ALU.max)
    TT(P, "dve", g4a, gl, b3(v1, 4), ALU.subtract)
    ACT(P, g4a, g4a, AF.Exp)
    RED(P, "dve", v2, g4a, ALU.add)
    P.op("dve", lambda e: e.reciprocal(out=v3, in_=v2), reads=[v2], writes=[v3])
    TT(P, "dve", g4a, g4a, b3(v3, 4), ALU.mult)
    TT(P, "dve", g4b, g4a, bc(bg.unsqueeze(1), [128, T, 4]), ALU.add)
    RED(P, "dve", v1, g4b, ALU.max)
    TT(P, "dve", goh, g4b, b3(v1, 4), ALU.is_equal)
    TT(P, "dve", g4b, goh, g4a, ALU.mult)
    RED(P, "dve", gw, g4b, ALU.add)
    TT(P, "dve", p48, el4, bc(goh.unsqueeze(3), [128, T, 4, 8]), ALU.mult)
    RED(P, "dve", e8a, p48.rearrange("p t g e -> p t e g"), ALU.add)
    RED(P, "dve", v1, e8a, ALU.max)
    TT(P, "dve", e8a, e8a, b3(v1, 8), ALU.subtract)
    ACT(P, e8a, e8a, AF.Exp)
    RED(P, "dve", v2, e8a, ALU.add)
    P.op("dve", lambda e: e.reciprocal(out=v3, in_=v2), reads=[v2], writes=[v3])
    TT(P, "dve", ep, e8a, b3(v3, 8), ALU.mult)
    TT(P, "dve", p48, bc(goh.unsqueeze(3), [128, T, 4, 8]), bc(be.rearrange("p (g e) -> p g e", g=4).unsqueeze(1), [128, T, 4, 8]), ALU.mult)
    RED(P, "dve", e8b, p48.rearrange("p t g e -> p t e g"), ALU.add)
    TT(P, "dve", e8b, e8b, ep, ALU.add)
    RED(P, "dve", v1, e8b, ALU.max)
    TT(P, "dve", oh1, e8b, b3(v1, 8), ALU.is_equal)
    STT(P, "dve", e8b, oh1, -1e30, e8b, ALU.mult, ALU.add)
    RED(P, "dve", v1, e8b, ALU.max)
    TT(P, "dve", oh2, e8b, b3(v1, 8), ALU.is_equal)
    TT(P, "dve", oh1, oh1, oh2, ALU.add)
    TT(P, "dve", e8a, oh1, ep, ALU.mult)
    RED(P, "dve", v2, e8a, ALU.add)
    P.op("dve", lambda e: e.reciprocal(out=v3, in_=v2), reads=[v2], writes=[v3])
    TT(P, "dve", v3, v3, gw, ALU.mult)
    TT(P, "dve", e8a, e8a, b3(v3, 8), ALU.mult)
    C4 = C.rearrange("p t (g e) -> p t g e", g=4)
    TT(P, "dve", C4, bc(goh.unsqueeze(3), [128, T, 4, 8]), bc(e8a.unsqueeze(2), [128, T, 4, 8]), ALU.mult)


def emit_experts(E, d_wg, d_wu, d_wd, WR, C, g2row, actT, sbuf_s, tmpb, nexp=32, chunks=None, first=None):
    P = E.P
    NS = WR.shape[1]
    CH = CHUNKS if chunks is None else chunks

    def wslot(m):
        return WR[:, m % NS, :]

    def load(e):
        g = wslot(3 * e).rearrange("p (c f) -> p c f", c=8)
        u = wslot(3 * e + 1).rearrange("p (c f) -> p c f", c=8)
        d = wslot(3 * e + 2).rearrange("p (c f) -> p c f", c=4)
        P.dma("pool", g, d_wg[e].rearrange("(c p) f -> p c f", p=128))
        P.dma("pool", u, d_wu[e].rearrange("(c p) f -> p c f", p=128))
        P.dma("pool", d, d_wd[e].rearrange("(c p) f -> p c f", p=128))
        return g, u, d

    pend = []
    cnt = [0]

    def gu(e, ci, g, u):
        t0, n = CH[ci]
        k = cnt[0] % 2
        a_t = actT[k]
        for fc in range(4):
            pg = E.pa[fc % 2]
            pu = E.pb[fc % 2]
            for c in range(8):
                MM(P, pg[:, 0:n], g[:, c, fc * 128:(fc + 1) * 128], E.AT[:, c, t0:t0 + n], c == 0, c == 7)
            for c in range(8):
                MM(P, pu[:, 0:n], u[:, c, fc * 128:(fc + 1) * 128], E.AT[:, c, t0:t0 + n], c == 0, c == 7)
            s = sbuf_s[fc % 2]
            ACT(P, s[:, 0:n], pg[:, 0:n], AF.Silu)
            TT(P, "dve", a_t[:, fc, 0:n], s[:, 0:n], pu[:, 0:n], ALU.mult)
        cnt[0] += 1
        return a_t

    ycnt = [0]

    def down(e, ci, d, a_t):
        t0, n = CH[ci]
        for tt in range(n // 128):
            tile = t0 // 128 + tt
            nn = 0 if tile < 16 else 1
            k = ycnt[0] % 2
            ycnt[0] += 1
            py = E.py[k]
            for half in range(2):
                for fc in range(4):
                    MM(P, py[:, half * 512:(half + 1) * 512], a_t[:, fc, tt * 128:(tt + 1) * 128], d[:, fc, half * 512:(half + 1) * 512], fc == 0, fc == 3)
            tm = tmpb[k]
            STT(P, "dve", tm, py[:], C[:, tile, e:e + 1], g2row[:, nn, :], ALU.mult, ALU.mult)
            TT(P, "dve", E.X[:, tile, :], E.X[:, tile, :], tm, ALU.add)

    if first == "prefetch":
        return load(0)
    nxt = load(0) if first is None else first
    for e in range(nexp):
        g, u, d = nxt
        for ci in range(len(CH)):
            a_t = gu(e, ci, g, u)
            if pend:
                pend.pop(0)()
            pend.append(lambda e=e, ci=ci, d=d, a_t=a_t: down(e, ci, d, a_t))
            if ci == 0 and e + 1 < nexp:
                nxt = load(e + 1)
    while pend:
        pend.pop(0)()


def phase_moe(E, d, nexp=32, chunks=None):
    P = E.P
    E.AT = E.sb("AT", [128, 8, NTOK], BF16)
    bmodT = E.sb("bmodT", [128, 48], F32)
    gffn = E.sb("gffn", [128, 8], F32)
    Wr = E.sb("Wr", [128, 8, 36], F32)
    bg = E.sb("bg", [128, 4], F32)
    be = E.sb("be", [128, 32], F32)
    for t, s in ((bmodT, d["bmodT"]), (gffn, d["gffn"]), (Wr, d["wr"]), (bg, d["bg"]), (be, d["be"])):
        P.dma("sp", t[:], s)
    WR = E.sb("WR", [128, 6, 4096], BF16)
    SCR = E.sb("SCR", [128, 10240], BF16)
    gs2 = E.sb("gs2", [128, 8, 2], F32)
    LG = E.sb("LG", [128, NT, 36], F32)
    C = E.sb("C", [128, NT, 32], F32)
    g2row = E.sb("g2row", [128, 2, 1024], F32)
    diag = E.sb("diag", [128, 2, 128], F32)
    actT = [E.sb("actT%d" % i, [128, 4, 512], BF16) for i in range(2)]
    sbuf_s = [E.sb("ssilu%d" % i, [128, 512], BF16) for i in range(2)]
    tmpb = [E.sb("tmpb%d" % i, [128, 1024], F32) for i in range(2)]

    def scr(boff, shape, dt):
        return aview(SCR, boff, shape, dt)

    wm = aview(SCR, 0, [8, 512], F32)
    first = emit_experts(E, d["w_gate"], d["w_up"], d["w_down"], WR, C[:], g2row, actT, sbuf_s, tmpb, first="prefetch")
    modT = get_mod(E, d["lay"], d["w_mod"], 24, 24, wm)
    TS(P, "dve", gs2[:], modT[:, 8:16, :], 1.0, ALU.add)
    TT(P, "dve", gs2[:], gs2[:], bc(gffn[:].unsqueeze(2), [128, 8, 2]), ALU.mult)
    for n in range(2):
        emit_rowbc(E, modT[:, 16:24, n], g2row[:, n, :], diag)
    if "dbg" in d:
        P.dma("sp", d["dbg"][:, 0:48], modT.rearrange("p a b -> p (a b)"))
        P.dma("sp", d["dbg"][:, 48:64], gs2.rearrange("p a b -> p (a b)"))
        P.dma("sp", d["dbg"][:, 64:2112], g2row.rearrange("p a b -> p (a b)"))
    emit_norm(E, std_tiles(E, LG), gs2, modT[:, 0:8, :], scr, Wr=Wr)
    if "dbg" in d:
        P.dma("sp", d["dbg"][:, 2112:2112 + NT * 36], LG.rearrange("p a b -> p (a b)"))
    emit_routing(E, LG[:], bg[:], be[:], C[:], scr)
    if "dbg" in d:
        P.dma("sp", d["dbg"][:, 2724:2724 + NT * 32], C.rearrange("p a b -> p (a b)"))
    emit_experts(E, d["w_gate"], d["w_up"], d["w_down"], WR, C[:], g2row, actT, sbuf_s, tmpb, nexp=nexp, chunks=chunks, first=first)


def mix_prologue(E, d, scr_t, want_g1row=True, want_norm=True):
    P = E.P
    bmodT = E.sb("bmodT", [128, 48], F32)
    gmix = E.sb("gmix", [128, 8], F32)
    P.dma("sp", bmodT, d["bmodT"])
    P.dma("sp", gmix, d["gmix"])
    gs1 = E.sb("gs1", [128, 8, 2], F32)
    wm = aview(scr_t, 0, [8, 512], F32)
    jc0 = 0 if want_norm else 16
    njc = (24 if want_g1row else 16) - jc0
    get_mod(E, d["lay"], d["w_mod"], jc0, njc, wm)
    modT = E.modAll[:, d["lay"], 0:24, :]
    if want_norm:
        TS(P, "dve", gs1, modT[:, 8:16, :], 1.0, ALU.add)
        TT(P, "dve", gs1, gs1, bc(gmix.unsqueeze(2), [128, 8, 2]), ALU.mult)
    g1row = None
    if want_g1row:
        g1row = E.sb("g1row", [128, 2, 1024], F32)
        diag = E.sb("diag1", [128, 2, 128], F32)
        for n in range(2):
            emit_rowbc(E, modT[:, 16:24, n], g1row[:, n, :], diag)
    return modT, gs1, g1row


def emit_resid(E, tile, py, grow, tmp):
    P = E.P
    nn = 0 if tile < 16 else 1
    TT(P, "dve", tmp, py, grow[:, nn, :], ALU.mult)
    TT(P, "dve", E.X[:, tile, :], E.X[:, tile, :], tmp, ALU.add)


def phase_conv(E, d):
    P = E.P
    E.AT = E.sb("AT", [128, 8, NTOK], BF16)
    SCR = E.sb("SCRc", [128, 10240], BF16)

    def scr(boff, shape, dt):
        return aview(SCR, boff, shape, dt)

    cw = E.sb("cw", [128, 8, 3], F32)
    hmask = E.sb("hmask", [128, 4], F32)
    XH = E.sb("XH", [128, 1024], F32)
    ATh = E.sb("ATh", [128, 8, 4], BF16)
    P.dma("sp", cw, d["cw"])
    P.dma("sp", hmask, d["hmask"])
    MEMSET(P, "dve", XH, 0.0)
    for i, src in enumerate(d["x_halo_rows"]):
        P.dma("sp", XH[i:i + 1, :], src)
    modT, gs1, g1row = mix_prologue(E, d, SCR)
    tiles = std_tiles(E) + [dict(src=XH, dst=ATh, groups=[(0, 2, 0), (2, 4, 1)])]
    emit_norm(E, tiles, gs1, modT[:, 0:8, :], scr)

    wout = E.sb("wout", [128, 8, 1024], BF16)
    P.dma("pool", wout, d["w_out"].rearrange("(c p) j -> p c j", p=128))
    bzT = E.sb("bzT", [128, 8, NTOK], BF16)
    win = [E.sb("win%d" % i, [128, 3, 8, 128], BF16) for i in range(2)]
    tmp = E.sb("tmpc", [128, 1024], F32)
    CU = scr(0, [2180], F32)
    bsb = scr(8736, [NTOK], BF16)
    csb = [scr(13088 + i * 2048, [512], F32) for i in range(2)]
    zc = scr(17184, [512], F32)
    chh = scr(19232, [4], F32)
    cuh = scr(19264, [4], F32)
    wsrc = d["w_in"].rearrange("(c p) j -> p c j", p=128)
    for fc in range(8):
        wb = win[fc % 2]
        for k in range(3):
            P.dma("pool", wb[:, k, :, :], wsrc[:, :, k * 1024 + fc * 128:k * 1024 + (fc + 1) * 128])
        ph = E.pa[0]
        for c in range(8):
            MM(P, ph[:, 0:4], wb[:, 1, c, :], ATh[:, c, :], c == 0, c == 7)
        for c in range(8):
            MM(P, ph[:, 4:8], wb[:, 2, c, :], ATh[:, c, :], c == 0, c == 7)
        COPY(P, "act", chh, ph[:, 0:4])
        TT(P, "dve", cuh, chh, ph[:, 4:8], ALU.mult)
        TT(P, "dve", cuh, cuh, hmask, ALU.mult)
        COPY(P, "dve", CU[:, 0:1], cuh[:, 0:1])
        COPY(P, "dve", CU[:, 2049:2051], cuh[:, 1:3])
        COPY(P, "dve", CU[:, 2179:2180], cuh[:, 3:4])
        for ci, (t0, n) in enumerate(CHUNKS):
            pb_ = E.pa[1]
            pc_ = E.pb[ci % 2]
            pu_ = E.py[ci % 2]
            for c in range(8):
                MM(P, pb_[:, 0:n], wb[:, 0, c, :], E.AT[:, c, t0:t0 + n], c == 0, c == 7)
            for c in range(8):
                MM(P, pc_[:, 0:n], wb[:, 1, c, :], E.AT[:, c, t0:t0 + n], c == 0, c == 7)
            for c in range(8):
                MM(P, pu_[:, 0:n], wb[:, 2, c, :], E.AT[:, c, t0:t0 + n], c == 0, c == 7)
            COPY(P, "act", bsb[:, t0:t0 + n], pb_[:, 0:n])
            cs = csb[ci % 2]
            COPY(P, "act", cs[:, 0:n], pc_[:, 0:n])
            o = 1 + t0 if ci < 4 else 2051
            TT(P, "dve", CU[:, o:o + n], cs[:, 0:n], pu_[:, 0:n], ALU.mult)
        for ci, (t0, n) in enumerate(CHUNKS):
            o = 1 + t0 if ci < 4 else 2051
            TS(P, "dve", zc[:, 0:n], CU[:, o:o + n], cw[:, fc, 1:2], ALU.mult)
            STT(P, "dve", zc[:, 0:n], CU[:, o - 1:o - 1 + n], cw[:, fc, 0:1], zc[:, 0:n], ALU.mult, ALU.add)
            STT(P, "dve", zc[:, 0:n], CU[:, o + 1:o + 1 + n], cw[:, fc, 2:3], zc[:, 0:n], ALU.mult, ALU.add)
            TT(P, "dve", bzT[:, fc, t0:t0 + n], bsb[:, t0:t0 + n], zc[:, 0:n], ALU.mult)
    for t in range(NT):
        py = E.py[t % 2]
        for half in range(2):
            for fc in range(8):
                MM(P, py[:, half * 512:(half + 1) * 512], bzT[:, fc, t * 128:(t + 1) * 128], wout[:, fc, half * 512:(half + 1) * 512], fc == 0, fc == 7)
        emit_resid(E, t, py, g1row, tmp)


def emit_rope(E, out, x, C2, S2, nh, half, tmp1, tmp2):
    P = E.P
    hd = 4 * half
    xv = x.rearrange("p (h a j f) -> p h a j f", h=nh, a=2, j=2)
    t2 = tmp2.rearrange("p (h a j f) -> p h a j f", h=nh, a=2, j=2)
    Cb = bc(C2.unsqueeze(1), [128, nh, hd])
    Sv = S2.rearrange("p (a j f) -> p a j f", a=2, j=2)
    TT(P, "dve", tmp1.rearrange("p (h d) -> p h d", h=nh), x.rearrange("p (h d) -> p h d", h=nh), Cb, ALU.mult)
    for j in range(2):
        TT(P, "pool", t2[:, :, :, j, :], xv[:, :, :, 1 - j, :], bc(Sv[:, :, j, :].unsqueeze(1), [128, nh, 2, half]), ALU.mult)
    TT(P, "dve", out, tmp1, tmp2, ALU.add)


def emit_headnorm(E, qn, ps, nh, hd, grow, scr4):
    P = E.P
    sq, ss, rs = scr4
    ACT(P, qn, ps, AF.Copy)
    ACT(P, sq, ps, AF.Square)
    RED(P, "dve", ss, sq.rearrange("p (h d) -> p h d", h=nh), ALU.add)
    TS(P, "dve", ss, ss, 1.0 / hd, ALU.mult, EPS, ALU.add)
    ACT(P, rs, ss, AF.Sqrt)
    P.op("dve", lambda e: e.reciprocal(out=ss, in_=rs), reads=[rs], writes=[ss])
    q3 = qn.rearrange("p (h d) -> p h d", h=nh)
    TT(P, "dve", q3, q3, bc(ss.unsqueeze(2), [128, nh, hd]), ALU.mult)
    TT(P, "dve", q3, q3, bc(grow.unsqueeze(1), [128, nh, hd]), ALU.mult)


def phase_gqa_proj(E, d):
    P = E.P
    E.AT = E.sb("AT", [128, 8, NTOK], BF16)
    SCR = E.sb("SCRa", [128, 10240], BF16)

    def scr(boff, shape, dt):
        return aview(SCR, boff, shape, dt)

    modT, gs1, _ = mix_prologue(E, d, SCR, want_g1row=False)
    emit_norm(E, std_tiles(E), gs1, modT[:, 0:8, :], scr)
    identb = E.sb("identb", [128, 128], BF16)
    COPY(P, "dve", identb, E.ident)
    gq = E.sb("gq", [128, 128], F32)
    gk = E.sb("gk", [128, 128], F32)
    P.dma("sp", gq, d["gq"])
    P.dma("sp", gk, d["gk"])
    wb = [E.sb("wqkv%d" % i, [128, 8, 512], BF16) for i in range(2)]
    stg = [E.sb("stg%d" % i, [128, 17 * 512], BF16) for i in range(2)]
    rC = [E.sb("ropeC%d" % i, [128, 128], F32) for i in range(2)]
    rS = [E.sb("ropeS%d" % i, [128, 128], F32) for i in range(2)]
    sets = []
    for i in range(2):
        o = i * 10240
        sets.append(dict(qn=scr(o, [512], F32), sq=scr(o + 2048, [512], F32), t1=scr(o + 4096, [512], F32),
                         t2=scr(o + 6144, [512], F32), qr=scr(o + 8192, [512], BF16), ss=scr(o + 9216, [4], F32),
                         rs=scr(o + 9248, [4], F32)))
    wsrc = d["w_qkv"].rearrange("(c p) j -> p c j", p=128)
    it = 0
    for k, cb in enumerate([4, 5, 6, 7, 0, 1, 2, 3]):
        w = wb[k % 2]
        P.dma("pool", w, wsrc[:, :, cb * 512:(cb + 1) * 512])
        sg = stg[k % 2]
        def stage1(t, S):
            ps = E.pa[t % 2]
            for c in range(8):
                MM(P, ps, E.AT[:, c, t * 128:(t + 1) * 128], w[:, c, :], c == 0, c == 7)
            if cb >= 6:
                COPY(P, "act", sg.rearrange("p (t c) -> p t c", t=17)[:, t, :], ps)
                return
            emit_headnorm(E, S["qn"], ps, 4, 128, gq if cb < 4 else gk, (S["sq"], S["ss"], S["rs"]))
            P.dma("sp", rC[t % 2], d["ropeC"][t])
            P.dma("sp", rS[t % 2], d["ropeS"][t])

        def stage2(t, S):
            if cb >= 6:
                return
            emit_rope(E, S["qr"], S["qn"], rC[t % 2], rS[t % 2], 4, 32, S["t1"], S["t2"])
            pt = E.pb[t % 2].bitcast(BF16)
            for h in range(4):
                TR(P, pt[:, h * 128:(h + 1) * 128], S["qr"][:, h * 128:(h + 1) * 128], identb)
            COPY(P, "act", sg.rearrange("p (h t) -> p h t", h=4)[:, :, t * 128:(t + 1) * 128], pt[:, 0:512].rearrange("p (h t) -> p h t", h=4))

        prev = None
        for t in range(NT):
            S = sets[it % 2]
            it += 1
            stage1(t, S)
            if prev is not None:
                stage2(*prev)
            prev = (t, S)
        stage2(*prev)
        if cb < 4:
            P.dma("sp", d["qT"][4 * cb:4 * cb + 4].rearrange("h p t -> p h t"), sg.rearrange("p (h t) -> p h t", h=4))
        elif cb < 6:
            P.dma("sp", d["kT"][4 * (cb - 4):4 * (cb - 4) + 4].rearrange("h p t -> p h t"), sg.rearrange("p (h t) -> p h t", h=4))
        else:
            P.dma("sp", d["v"].rearrange("(t p) c -> p t c", p=128)[:, :, (cb - 6) * 512:(cb - 5) * 512], sg.rearrange("p (t c) -> p t c", t=17))
        if cb == 7 and "gather" in d:
            d["gather"]()


def phase_attn(E, d, Hk, G, has_pe, scale, scr_t):
    P = E.P
    H = Hk * G
    KT = [E.sb("KT%d" % i, [128, NKEY], BF16) for i in range(2)]
    VG = [E.sb("VG%d" % i, [128, NKT, 128], BF16) for i in range(2)]
    QT = [E.sb("QT%d" % i, [128, NTOK], BF16) for i in range(2)]
    OTs = [E.sb("OTs%d" % i, [128, NTOK], BF16) for i in range(2)]
    PT = [E.sb("PT%d" % i, [128, 512], BF16) for i in range(3)] + [aview(scr_t, 16384 + i * 1024, [512], BF16) for i in range(2)]
    NPT = len(PT)
    accA = [aview(scr_t, i * 2048, [512], F32) for i in range(2)]
    accB = [aview(scr_t, 4096 + i * 2048, [512], F32) for i in range(2)]
    rl = [E.sb("rl%d" % i, [128, 512], F32) for i in range(2)]
    ones_b = E.sb("ones_b", [128, 128], BF16)
    MEMSET(P, "dve", ones_b, 1.0)
    if has_pe:
        KPE = E.sb("KPE", [128, NKEY], BF16)
        P.dma("sp", KPE.rearrange("p (r t) -> p r t", r=2), d["kpeT_all"])
        QPE = [E.sb("QPE%d" % i, [128, NTOK], BF16) for i in range(2)]
    ptc = [0]
    cc = [0]
    for g in range(Hk):
        kt_ = KT[g % 2]
        vg = VG[g % 2]
        P.dma("sp", kt_.rearrange("p (r t) -> p r t", r=2), d["kT_all"][g])
        for (t0, n, vgc) in d["v_chunks"]:
            for r in range(2):
                k0 = r * NT + t0 // 128
                P.dma("sp", vg[:, k0:k0 + n // 128, :], vgc[r * n:(r + 1) * n, g * 128:(g + 1) * 128].rearrange("(t p) c -> p t c", p=128))
        for gi in range(G):
            h = g * G + gi
            qt = QT[h % 2]
            ot = OTs[h % 2]
            P.dma("sp", qt, d["qT"][h])
            if has_pe:
                pb_ = (h % 2) * 64
                if h % 2 == 0:
                    qpe = QPE[(h // 2) % 2]
                    P.dma("sp", qpe, d["qpeT"][h // 2])
            for ci, (t0, n) in enumerate(CHUNKS):
                kts = list(range(NKT)) if ci < 4 else [16, 33]
                po = E.pb[cc[0] % 2]
                pl = E.py[cc[0] % 2]

                def qk(kt):
                    ps = E.pa[kt % 2] if ci < 4 else E.pa[kts.index(kt) % 2]
                    MM(P, ps[:, 0:n], kt_[:, kt * 128:(kt + 1) * 128], qt[:, t0:t0 + n], True, not has_pe)
                    if has_pe:
                        MM(P, ps[:, 0:n], KPE[pb_:pb_ + 64, kt * 128:(kt + 1) * 128], qpe[pb_:pb_ + 64, t0:t0 + n], False, True)
                    pt_ = PT[ptc[0] % NPT]
                    ptc[0] += 1
                    ACT(P, pt_[:, 0:n], ps[:, 0:n], AF.Exp, scale=scale)
                    return pt_

                def pv(kt, pt_, first, last, idx):
                    MM(P, po[:, 0:n], vg[:, kt, :], pt_[:, 0:n], first, last)
                    MM(P, pl[:, 0:n], ones_b, pt_[:, 0:n], first, last)

                prev = None
                for i, kt in enumerate(kts):
                    cur = (kt, qk(kt))
                    if prev is not None:
                        pv(prev[0], prev[1], i == 1, False, i - 1)
                    prev = cur
                pv(prev[0], prev[1], len(kts) == 1, True, len(kts) - 1)
                cc[0] += 1
                r = rl[ci % 2]
                P.op("dve", lambda e, r=r, pl=pl, n=n: e.reciprocal(out=r[:, 0:n], in_=pl[:, 0:n]), reads=[pl[:, 0:n]], writes=[r[:, 0:n]])
                TT(P, "dve", ot[:, t0:t0 + n], po[:, 0:n], r[:, 0:n], ALU.mult)
                for _ in range(d.get("bg_per_slot", 0)):
                    if E.bg:
                        E.bg.pop(0)()
            P.dma("sp", d["oT"][h], ot)


def phase_oproj(E, d, H, g1row):
    P = E.P
    wo = E.sb("wo", [128, H, 1024], BF16)
    P.dma("pool", wo, d["w_o"].rearrange("(h p) j -> p h j", p=128))
    otl = [E.sb("otl%d" % i, [128, H, 128], BF16) for i in range(2)]
    tmp = E.sb("tmpo", [128, 1024], F32)
    osrc = d["oT"].rearrange("h p t -> p h t")
    for t in range(NT):
        o = otl[t % 2]
        P.dma("sp", o, osrc[:, :, t * 128:(t + 1) * 128])
        py = E.py[t % 2]
        for half in range(2):
            for h in range(H):
                MM(P, py[:, half * 512:(half + 1) * 512], o[:, h, :], wo[:, h, half * 512:(half + 1) * 512], h == 0, h == H - 1)
        emit_resid(E, t, py, g1row, tmp)


def phase_mla_proj(E, d):
    P = E.P
    E.AT = E.sb("AT", [128, 8, NTOK], BF16)
    SCR = E.sb("SCRm", [128, 10240], BF16)

    def scr(boff, shape, dt):
        return aview(SCR, boff, shape, dt)

    modT, gs1, _ = mix_prologue(E, d, SCR, want_g1row=False)
    emit_norm(E, std_tiles(E), gs1, modT[:, 0:8, :], scr)
    identb = E.sb("identb", [128, 128], BF16)
    COPY(P, "dve", identb, E.ident)
    gq = E.sb("gqm", [128, 512], F32)
    gkv = E.sb("gkvm", [128, 256], F32)
    P.dma("sp", gq, d["gq"])
    P.dma("sp", gkv, d["gkv"])
    wdkv = E.sb("wdkv", [128, 8, 320], BF16)
    wdq = E.sb("wdq", [128, 8, 512], BF16)
    wuqr = E.sb("wuqr", [128, 4, 512], BF16)
    wuqn = E.sb("wuqn", [128, 4, 1024], BF16)
    wukk = E.sb("wukk", [128, 2, 1024], BF16)
    wukv = E.sb("wukv", [128, 2, 1024], BF16)
    P.dma("pool", wdkv, d["w_dkv"].rearrange("(c p) j -> p c j", p=128))
    P.dma("pool", wdq, d["w_dq"].rearrange("(c p) j -> p c j", p=128))
    P.dma("pool", wuqr, d["w_uq_rope"].rearrange("(c p) j -> p c j", p=128))
    P.dma("pool", wuqn, d["w_uq_nope"].rearrange("(c p) j -> p c j", p=128))
    P.dma("pool", wukk, d["w_ukv_k"].rearrange("(c p) j -> p c j", p=128))
    P.dma("pool", wukv, d["w_ukv_v"].rearrange("(c p) j -> p c j", p=128))
    ckvT = E.sb("ckvT", [128, 2, NTOK], BF16)
    cqT = E.sb("cqT", [128, 4, NTOK], BF16)
    rC = [E.sb("ropeCm%d" % i, [128, 64], F32) for i in range(2)]
    rS = [E.sb("ropeSm%d" % i, [128, 64], F32) for i in range(2)]
    qpes = [E.sb("qpes%d" % i, [128, 4, 128], BF16) for i in range(2)]
    kpes = [E.sb("kpes%d" % i, [128, 128], BF16) for i in range(2)]
    stg = [E.sb("stgm%d" % i, [128, NTOK], BF16) for i in range(2)]
    vst = [E.sb("vst%d" % i, [128, 1024], BF16) for i in range(2)]
    kvs, qss = [], []
    for i in range(2):
        o = i * 10240
        kvs.append(dict(cn=scr(o, [256], F32), sq=scr(o + 1024, [256], F32), kpf=scr(o + 2048, [64], F32),
                        kt1=scr(o + 2304, [64], F32), kt2=scr(o + 2560, [64], F32), kpb=scr(o + 2816, [2, 64], BF16),
                        cb16=scr(o + 3072, [256], BF16), ss=scr(o + 3584, [1], F32), rs=scr(o + 3616, [1], F32)))
        qss.append(dict(qn=scr(o, [512], F32), sq=scr(o + 2048, [512], F32), qb16=scr(o + 4096, [512], BF16),
                        qpf=scr(o + 5120, [512], F32), qt1=scr(o + 2048, [512], F32), qt2=scr(o + 7168, [512], F32),
                        qpb=scr(o + 9216, [512], BF16), ss=E.sb("qss%d" % i, [128, 1], F32), rs=E.sb("qrs%d" % i, [128, 1], F32)))
    def kv1(t):
        S = kvs[t % 2]
        tc_ = slice(t * 128, (t + 1) * 128)
        pkv = E.pa[t % 2]
        for c in range(8):
            MM(P, pkv[:, 0:320], E.AT[:, c, tc_], wdkv[:, c, :], c == 0, c == 7)
        P.dma("sp", rC[t % 2], d["ropeC"][t])
        P.dma("sp", rS[t % 2], d["ropeS"][t])
        ACT(P, S["kpf"], pkv[:, 256:320], AF.Copy)
        emit_headnorm(E, S["cn"], pkv[:, 0:256], 1, 256, gkv, (S["sq"], S["ss"], S["rs"]))
        COPY(P, "dve", S["cb16"], S["cn"])

    def kv2(t):
        S = kvs[t % 2]
        tc_ = slice(t * 128, (t + 1) * 128)
        ptr = E.pb[t % 2].bitcast(BF16)
        emit_rope(E, S["kpb"][:, 0, :], S["kpf"], rC[t % 2], rS[t % 2], 1, 16, S["kt1"], S["kt2"])
        COPY(P, "dve", S["kpb"][:, 1, :], S["kpb"][:, 0, :])
        for kc in range(2):
            TR(P, ptr[:, kc * 128:(kc + 1) * 128], S["cb16"][:, kc * 128:(kc + 1) * 128], identb)
        TR(P, ptr[:, 256:384], S["kpb"].rearrange("p a b -> p (a b)"), identb)
        COPY(P, "act", ckvT[:, :, tc_], ptr[:, 0:256].rearrange("p (a b) -> p a b", a=2))
        kp = kpes[t % 2]
        COPY(P, "act", kp, ptr[:, 256:384])
        P.dma("sp", d["kpeT"][:, tc_], kp)
        pv_ = E.py[t % 2]
        for half in range(2):
            for kc in range(2):
                MM(P, pv_[:, half * 512:(half + 1) * 512], ckvT[:, kc, tc_], wukv[:, kc, half * 512:(half + 1) * 512], kc == 0, kc == 1)
        vs = vst[t % 2]
        COPY(P, "act", vs, pv_)
        P.dma("sp", d["v"][t * 128:(t + 1) * 128, :], vs)

    for t in range(NT):
        kv1(t)
        if t > 0:
            kv2(t - 1)
    kv2(NT - 1)
    k = 0

    def upproj(h, which):
        nonlocal k
        sg = stg[k % 2]
        k += 1
        for ci, (t0, n) in enumerate(CHUNKS):
            ps = E.pa[ci % 2]
            if which == 0:
                for kc in range(2):
                    MM(P, ps[:, 0:n], wukk[:, kc, h * 128:(h + 1) * 128], ckvT[:, kc, t0:t0 + n], kc == 0, kc == 1)
            else:
                for qc in range(4):
                    MM(P, ps[:, 0:n], wuqn[:, qc, h * 128:(h + 1) * 128], cqT[:, qc, t0:t0 + n], qc == 0, qc == 3)
            COPY(P, "act" if ci % 2 == 0 else "dve", sg[:, t0:t0 + n], ps[:, 0:n])
        P.dma("sp", (d["kT"] if which == 0 else d["qT"])[h], sg)

    for h in range(8):
        upproj(h, 0)
    if "gather" in d:
        d["gather"]()
    def q1(t):
        S = qss[t % 2]
        tc_ = slice(t * 128, (t + 1) * 128)
        pq = E.pb[t % 2]
        ptr = E.py[t % 2].bitcast(BF16)
        for c in range(8):
            MM(P, pq, E.AT[:, c, tc_], wdq[:, c, :], c == 0, c == 7)
        emit_headnorm(E, S["qn"], pq, 1, 512, gq, (S["sq"], S["ss"], S["rs"]))
        COPY(P, "dve", S["qb16"], S["qn"])
        for qc in range(4):
            TR(P, ptr[:, qc * 128:(qc + 1) * 128], S["qb16"][:, qc * 128:(qc + 1) * 128], identb)
        COPY(P, "act", cqT[:, :, tc_], ptr[:, 0:512].rearrange("p (a b) -> p a b", a=4))
        pqp = E.pa[t % 2]
        for qc in range(4):
            MM(P, pqp, cqT[:, qc, tc_], wuqr[:, qc, :], qc == 0, qc == 3)
        ACT(P, S["qpf"], pqp, AF.Copy)
        P.dma("sp", rC[t % 2], d["ropeC"][t])
        P.dma("sp", rS[t % 2], d["ropeS"][t])

    def q2(t):
        S = qss[t % 2]
        tc_ = slice(t * 128, (t + 1) * 128)
        ptr = E.py[t % 2].bitcast(BF16)
        emit_rope(E, S["qpb"], S["qpf"], rC[t % 2], rS[t % 2], 8, 16, S["qt1"], S["qt2"])
        for pr in range(4):
            TR(P, ptr[:, 1024 + pr * 128:1024 + (pr + 1) * 128], S["qpb"][:, pr * 128:(pr + 1) * 128], identb)
        qs = qpes[t % 2]
        COPY(P, "act", qs, ptr[:, 1024:1536].rearrange("p (a b) -> p a b", a=4))
        P.dma("sp", d["qpeT"].rearrange("h p t -> p h t")[:, :, tc_], qs)

    for t in range(NT):
        q1(t)
        if t > 0:
            q2(t - 1)
    q2(NT - 1)
    for h in range(8):
        upproj(h, 1)


def phase_attnB(E, d, Hk, G, has_pe, scale):
    SCR = E.sb("SCRb", [128, 10240], BF16)
    modT, gs1, g1row = mix_prologue(E, d, SCR, want_norm=False)
    if d.get("bg_sched"):
        stage = [aview(SCR, i * 8192, [8, 256], F32) for i in range(2)]
        E.bg = make_mod_jobs(E, d["bg_sched"], stage)
        slots = Hk * G * len(CHUNKS)
        d["bg_per_slot"] = -(-len(E.bg) // slots) if E.bg else 0
    phase_attn(E, d, Hk, G, has_pe, scale, SCR)
    while E.bg:
        E.bg.pop(0)()
    phase_oproj(E, d, Hk * G, g1row)


def phase_final(E, d):
    P = E.P
    gfin = E.sb("gfin", [128, 1024], F32)
    P.dma("sp", gfin, d["gfin"])
    junk = E.sb("junkf", [128, 1024], BF16)
    ss = E.sb("ssf", [128, 16], F32)
    sq = E.sb("sqf", [128, 16], F32)
    for t in range(16):
        ACT(P, junk, E.X[:, t, :], AF.Square, accum_out=ss[:, t:t + 1])
    TS(P, "dve", ss, ss, 1.0 / D, ALU.mult, EPS, ALU.add)
    ACT(P, sq, ss, AF.Sqrt)
    P.op("dve", lambda e: e.reciprocal(out=ss, in_=sq), reads=[sq], writes=[ss])
    for t in range(16):
        TS(P, "dve", E.X[:, t, :], E.X[:, t, :], ss[:, t:t + 1], ALU.mult)
        TT(P, "pool", E.X[:, t, :], E.X[:, t, :], gfin, ALU.mult)


MLA_SCALE = 192 ** -0.5
GQA_SCALE = 128 ** -0.5


def _mixd(E, lay):
    return {"lay": lay, "w_mod": E.din("w_mod_%d" % lay, [D, 6 * D]), "bmodT": E.din("bmodT_%d" % lay, [128, 48]),
            "gmix": E.din("gmix_%d" % lay, [128, 8])}


BG_SCHED = {0: [(0, 24, 24), (1, 0, 24), (1, 24, 24), (2, 0, 16)],
            2: [(2, 24, 24), (3, 0, 16), (3, 16, 8), (3, 24, 24)]}


def _kv_io(E, lay, fused, H, Hk, has_pe):
    mk = E.dscr if fused else E.dout
    io = {"qT": mk("qT_%d" % lay, [H, 128, NTOK], BF16), "kT": mk("kT_%d" % lay, [Hk, 128, NTOK], BF16),
          "v": mk("v_%d" % lay, [NTOK, Hk * 128], BF16)}
    if has_pe:
        io["qpeT"] = mk("qpeT_%d" % lay, [H // 2, 128, NTOK], BF16)
        io["kpeT"] = mk("kpeT_%d" % lay, [128, NTOK], BF16)
    return io


def _make_gather(E, lay, io, Hk, has_pe):
    def gather():
        kTg = []
        for g in range(Hk):
            kg = E.dscr("kTg_%d_%d" % (lay, g), [256, NTOK], BF16)
            ALLGATHER(E.P, kg, io["kT"][g])
            kTg.append(kg.rearrange("(r p) t -> p r t", r=2))
        vch = []
        for ci, (t0, n) in enumerate(CHUNKS):
            vgc = E.dscr("vg_%d_%d" % (lay, ci), [2 * n, Hk * 128], BF16)
            ALLGATHER(E.P, vgc, io["v"][t0:t0 + n, :])
            vch.append((t0, n, vgc))
        kv = {"kT_all": kTg, "v_chunks": vch}
        if has_pe:
            kpeg = E.dscr("kpeg_%d" % lay, [256, NTOK], BF16)
            ALLGATHER(E.P, kpeg, io["kpeT"])
            kv["kpeT_all"] = kpeg.rearrange("(r p) t -> p r t", r=2)
        if not hasattr(E, "kv"):
            E.kv = {}
        E.kv[lay] = kv
    return gather


def emit_phase(E, ph):
    kind = ph[0]
    if kind == "moe":
        lay = ph[1]
        d = {"lay": lay, "w_mod": E.din("w_mod_%d" % lay, [D, 6 * D]), "bmodT": E.din("bmodT_%d" % lay, [128, 48]),
             "gffn": E.din("gffn_%d" % lay, [128, 8]), "wr": E.din("wr_%d" % lay, [128, 8, 36]),
             "bg": E.din("bg_%d" % lay, [128, 4]), "be": E.din("be_%d" % lay, [128, 32]),
             "w_gate": E.din("w_gate_%d" % lay, [32, D, 512]), "w_up": E.din("w_up_%d" % lay, [32, D, 512]),
             "w_down": E.din("w_down_%d" % lay, [32, 512, D])}
        phase_moe(E, d, chunks=CHUNKS[:4] if lay == 3 else None)
    elif kind == "conv":
        lay = ph[1]
        P = E.P
        hsrc = E.dscr("hsrc_%d" % lay, [4, D])
        hgat = E.dscr("hgat_%d" % lay, [8, D])
        P.dma("sp", hsrc[0:1, :], E.X[0:1, 0, :])
        P.dma("sp", hsrc[1:2, :], E.X[127:128, 15, :])
        P.dma("sp", hsrc[2:3, :], E.X[0:1, 16, :])
        P.dma("sp", hsrc[3:4, :], E.X[127:128, 16, :])
        ALLGATHER(P, hgat, hsrc)
        d = _mixd(E, lay)
        d.update({"cw": E.din("cw_%d" % lay, [128, 8, 3]), "hmask": E.din("hmask", [128, 4]),
                  "x_halo_rows": [hgat[1:2, :], hgat[4:5, :], hgat[3:4, :], hgat[6:7, :]],
                  "w_in": E.din("w_in_%d" % lay, [D, 3 * D]), "w_out": E.din("w_out_%d" % lay, [D, D])})
        phase_conv(E, d)
    elif kind == "gqaA":
        lay = ph[1]
        d = _mixd(E, lay)
        d.update({"gq": E.din("gq_%d" % lay, [128, 128]), "gk": E.din("gk_%d" % lay, [128, 128]),
                  "ropeC": E.din("ropeC32", [NT, 128, 128]), "ropeS": E.din("ropeS32", [NT, 128, 128]),
                  "w_qkv": E.din("w_qkv_%d" % lay, [D, 4096])})
        io = _kv_io(E, lay, True, 16, 8, False)
        d.update(io)
        d["gather"] = _make_gather(E, lay, io, 8, False)
        phase_gqa_proj(E, d)
    elif kind == "mlaA":
        lay = ph[1]
        d = _mixd(E, lay)
        d.update({"gq": E.din("gq_%d" % lay, [128, 512]), "gkv": E.din("gkv_%d" % lay, [128, 256]),
                  "ropeC": E.din("ropeC16", [NT, 128, 64]), "ropeS": E.din("ropeS16", [NT, 128, 64]),
                  "w_dkv": E.din("w_dkv_%d" % lay, [D, 320]), "w_dq": E.din("w_dq_%d" % lay, [D, 512]),
                  "w_uq_rope": E.din("w_uq_rope_%d" % lay, [512, 512]), "w_uq_nope": E.din("w_uq_nope_%d" % lay, [512, 1024]),
                  "w_ukv_k": E.din("w_ukv_k_%d" % lay, [256, 1024]), "w_ukv_v": E.din("w_ukv_v_%d" % lay, [256, 1024])})
        io = _kv_io(E, lay, True, 8, 8, True)
        d.update(io)
        d["gather"] = _make_gather(E, lay, io, 8, True)
        phase_mla_proj(E, d)
    elif kind in ("gqaB", "mlaB"):
        lay = ph[1]
        Hk, G, has_pe, scale = (8, 2, False, GQA_SCALE) if kind == "gqaB" else (8, 1, True, MLA_SCALE)
        H = Hk * G
        d = _mixd(E, lay)
        io = _kv_io(E, lay, True, H, Hk, has_pe)
        d.update(E.kv[lay])
        d.update({"qT": io["qT"], "w_o": E.din("w_o_%d" % lay, [H * 128, D]), "oT": E.dscr("oT_%d" % lay, [H, 128, NTOK], BF16)})
        if has_pe:
            d["qpeT"] = io["qpeT"]
        d["bg_sched"] = [b for b in BG_SCHED.get(lay, []) if b[0] in E.lays]
        phase_attnB(E, d, Hk, G, has_pe, scale)
    elif kind == "final":
        phase_final(E, {"gfin": E.din("gfin", [128, 1024])})
    else:
        raise ValueError(kind)


_PROG_CACHE = {}


def build_program(phases):
    key = tuple(phases)
    if key in _PROG_CACHE:
        return _PROG_CACHE[key]
    nc = bass.Bass("TRN2", target_bir_lowering=False)
    st = ExitStack()
    with st:
        E = Env(nc, st)
        E.lays = sorted({p[1] for p in phases if len(p) > 1})
        setup_common(E)
        d_x = E.din("x_in", [NTOK, D])
        d_y = E.dout("x_out", [NTOK, D])
        load_x(E, d_x)
        for ph in phases:
            with E.phase():
                emit_phase(E, ph)
        store_x(E, d_y)
        E.P.barrier()
        E.P.emit()
    _PROG_CACHE[key] = nc
    return nc


def _fm(v):
    return np.ascontiguousarray(np.asarray(v, np.float32).reshape(-1, 128).T)


def _rows(v, n=128):
    v = np.asarray(v, np.float32)
    return np.ascontiguousarray(np.broadcast_to(v, (n,) + v.shape))


def _rope_tables(h, half):
    inv = (10000.0 ** (-np.arange(half, dtype=np.float32) / half)).astype(np.float32)
    C = np.ones((NT, 128, 2, 2, half), np.float32)
    S = np.zeros((NT, 128, 2, 2, half), np.float32)
    t = h * 2048 + np.arange(2048)
    for a, pos in enumerate((t // 64, t % 64)):
        ang = pos.astype(np.float32)[:, None] * inv[None, :]
        c = np.cos(ang).reshape(16, 128, half)
        s = np.sin(ang).reshape(16, 128, half)
        C[:16, :, a, 0] = c
        C[:16, :, a, 1] = c
        S[:16, :, a, 0] = -s
        S[:16, :, a, 1] = s
    return C.reshape(NT, 128, 4 * half), S.reshape(NT, 128, 4 * half)


def _layer_inputs(inp, ph):
    kind = ph[0]
    o = {}
    if kind == "final":
        o["gfin"] = _rows(inp["final_norm_g"])
        return o
    lay = ph[1]
    o["w_mod_%d" % lay] = inp["w_mod"][lay]
    o["bmodT_%d" % lay] = _fm(inp["b_mod"][lay])
    j = lay // 3
    if kind == "moe":
        wr = np.concatenate([inp["moe_w_group"][lay], inp["moe_w_expert"][lay]], 1)
        o["gffn_%d" % lay] = _fm(inp["norm_ffn_g"][lay])
        o["wr_%d" % lay] = np.ascontiguousarray(wr.reshape(8, 128, 36).transpose(1, 0, 2))
        o["bg_%d" % lay] = _rows(inp["moe_b_group"][lay])
        o["be_%d" % lay] = _rows(inp["moe_b_expert"][lay])
        o["w_gate_%d" % lay] = inp["moe_w_gate"][lay]
        o["w_up_%d" % lay] = inp["moe_w_up"][lay]
        o["w_down_%d" % lay] = inp["moe_w_down"][lay]
        return o
    o["gmix_%d" % lay] = _fm(inp["norm_mix_g"][lay])
    if kind == "conv":
        o["cw_%d" % lay] = np.ascontiguousarray(inp["conv_w"][j].reshape(3, 8, 128).transpose(2, 1, 0))
        o["w_in_%d" % lay] = inp["conv_w_in"][j]
        o["w_out_%d" % lay] = inp["conv_w_out"][j]
    elif kind == "gqaA":
        o["gq_%d" % lay] = _rows(inp["gqa_q_norm_g"][j])
        o["gk_%d" % lay] = _rows(inp["gqa_k_norm_g"][j])
        o["w_qkv_%d" % lay] = inp["gqa_w_qkv"][j]
    elif kind == "gqaB":
        o["w_o_%d" % lay] = inp["gqa_w_o"][j]
    elif kind == "mlaA":
        wuq = inp["mla_w_uq"][j].reshape(512, 8, 192)
        wukv = inp["mla_w_ukv"][j].reshape(256, 8, 256)
        o["gq_%d" % lay] = _rows(inp["mla_g_q"][j])
        o["gkv_%d" % lay] = _rows(inp["mla_g_kv"][j])
        o["w_dkv_%d" % lay] = inp["mla_w_dkv"][j]
        o["w_dq_%d" % lay] = inp["mla_w_dq"][j]
        o["w_uq_nope_%d" % lay] = np.ascontiguousarray(wuq[:, :, :128].reshape(512, 1024))
        o["w_uq_rope_%d" % lay] = np.ascontiguousarray(wuq[:, :, 128:].reshape(512, 512))
        o["w_ukv_k_%d" % lay] = np.ascontiguousarray(wukv[:, :, :128].reshape(256, 1024))
        o["w_ukv_v_%d" % lay] = np.ascontiguousarray(wukv[:, :, 128:].reshape(256, 1024))
    elif kind == "mlaB":
        o["w_o_%d" % lay] = inp["mla_w_o"][j]
    return o


def _core_tokens(xb, hb, h):
    return np.ascontiguousarray(np.concatenate([xb[h * 2048:(h + 1) * 2048], hb[h * 128:(h + 1) * 128]], 0))


def _run_group(phases, inp, xs, per_core_extra):
    nc = build_program(phases)
    shared = {"ident_in": np.eye(128, dtype=np.float32)}
    for ph in phases:
        shared.update(_layer_inputs(inp, ph))
    kinds = [p[0] for p in phases]
    in_maps = []
    for core in range(8):
        b, h = core // 2, core % 2
        m = dict(shared)
        m["cond_in"] = np.ascontiguousarray(np.stack([_fm(inp["c"][b]), _fm(inp["c_ctx"])], -1))
        m["x_in"] = xs[core]
        if "gqaA" in kinds:
            m["ropeC32"], m["ropeS32"] = _rope_tables(h, 32)
        if "mlaA" in kinds:
            m["ropeC16"], m["ropeS16"] = _rope_tables(h, 16)
        if "conv" in kinds:
            hm = np.zeros(4, np.float32)
            hm[[0, 2]] = 1.0 if h > 0 else 0.0
            hm[[1, 3]] = 1.0 if h < 1 else 0.0
            m["hmask"] = _rows(hm)
        m.update(per_core_extra[core])
        in_maps.append(m)
    res = run_bass_kernel_spmd(nc, in_maps, core_ids=list(range(8)))
    return res.results


ALL_PHASES = (("mlaA", 0), ("mlaB", 0), ("moe", 0), ("conv", 1), ("moe", 1), ("gqaA", 2), ("gqaB", 2), ("moe", 2),
              ("mlaA", 3), ("mlaB", 3), ("moe", 3), ("final",))


def kernel(**inputs):
    inp = {k: np.asarray(v) for k, v in inputs.items()}
    x, ctx = inp["x"].astype(np.float32, copy=False), inp["ctx"].astype(np.float32, copy=False)
    xs = [_core_tokens(x[c // 2], ctx[c // 2], c % 2) for c in range(8)]
    r = _run_group(ALL_PHASES, inp, xs, [{} for _ in range(8)])
    out = np.empty((4, 4096, D), np.float32)
    for c in range(8):
        out[c // 2, (c % 2) * 2048:(c % 2 + 1) * 2048] = r[c]["x_out"][:2048]
    return out
```
